# Optimizing a Trainium2 kernel written in Bass

```python
import jax
import jax.numpy as jnp
from jax import lax
import numpy as np

D_MODEL = 1024
BATCH = 8
SEQ = 4096
DEPTH = 2

CTX_LEN = 256
GRID_W = 64
N_MOD = 6
RMS_EPS = 1e-6
ROPE_BASE = 10000.0
HEAD_DIM = 64
F32 = jnp.float32

RWKV_HEADS = 4
RWKV_DIM = RWKV_HEADS * HEAD_DIM
DECAY_LORA = 64
AAA_LORA = 64
GATE_LORA = 128
RWKV_GN_EPS = 64e-5

MLA_HEADS = 8
MLA_NOPE = 64
MLA_ROPE = 32
MLA_QK = MLA_NOPE + MLA_ROPE
MLA_V = 64
MLA_Q_RANK = 256
MLA_KV_RANK = 128
MLA_DIM = MLA_HEADS * MLA_V
Q_BLOCK = 128

RET_HEADS = 4
RET_KEY = 64
RET_VAL = 64
RET_DIM = RET_HEADS * RET_VAL
RET_CHUNK = 64

N_GROUPS = 4
EXPERTS_PER_GROUP = 8
N_EXPERTS = N_GROUPS * EXPERTS_PER_GROUP
TOP_K = 2
EXPERT_HIDDEN = 512

IN_WIDTHS = (RWKV_DIM, RWKV_DIM, RWKV_DIM, 2 * DECAY_LORA, 2 * AAA_LORA, GATE_LORA,
             MLA_Q_RANK, MLA_KV_RANK, MLA_ROPE,
             RET_HEADS * RET_KEY, RET_HEADS * RET_KEY, RET_DIM, RET_DIM)
D_IN = sum(IN_WIDTHS)

kernel_name = 'hybrid_rwkv7_mla_retention_hmoe_dit'


def rmsnorm(x, w):
    xf = x.astype(F32)
    y = xf * lax.rsqrt(jnp.mean(xf * xf, axis=-1, keepdims=True) + RMS_EPS)
    return (y * w.astype(F32)).astype(x.dtype)


def split_columns(u):
    parts, start = [], 0
    for width in IN_WIDTHS:
        parts.append(u[..., start:start + width])
        start += width
    return parts


def axial_rope_table(n_tokens, rot_dim):
    rows = n_tokens // GRID_W
    row = jnp.repeat(jnp.arange(rows, dtype=F32), GRID_W)
    col = jnp.tile(jnp.arange(GRID_W, dtype=F32), rows)
    n_freq = rot_dim // 4
    inv_freq = ROPE_BASE ** (-jnp.arange(n_freq, dtype=F32) / n_freq)
    ang = jnp.concatenate([row[:, None] * inv_freq, col[:, None] * inv_freq], axis=-1)
    return jnp.cos(ang), jnp.sin(ang)


def apply_rope(x, table):
    if table is None:
        return x
    cos, sin = table
    cos = cos[None, :, None, :]
    sin = sin[None, :, None, :]
    xf = x.astype(F32)
    half = x.shape[-1] // 2
    x1, x2 = xf[..., :half], xf[..., half:]
    return jnp.concatenate([x1 * cos - x2 * sin, x1 * sin + x2 * cos], axis=-1).astype(x.dtype)


def centred_conv3(u, w):
    up = jnp.pad(u, ((0, 0), (1, 1), (0, 0)))
    return up[:, :-2] * w[0] + up[:, 1:-1] * w[1] + up[:, 2:] * w[2]


def rwkv_prepare(parts, p):
    r, k, v, w_dn, a_dn, g_dn = parts
    B, L, _ = r.shape
    heads = lambda t: t.astype(F32).reshape(B, L, RWKV_HEADS, HEAD_DIM)
    r, k, v = jnp.split(centred_conv3(jnp.concatenate([r, k, v], axis=-1), p['rwkv_conv']), 3, axis=-1)
    kk = heads(k * p['rwkv_k_k'])
    kk = kk / jnp.maximum(jnp.linalg.norm(kk, axis=-1, keepdims=True), 1e-12)
    g = jax.nn.sigmoid(g_dn) @ p['rwkv_g_up']
    per_dir = []
    for d in range(2):
        w_lora = jnp.tanh(w_dn[..., d * DECAY_LORA:(d + 1) * DECAY_LORA]) @ p['rwkv_w_up'][d]
        log_w = -jax.nn.softplus(-(p['rwkv_w0'][d] + w_lora).astype(F32)) - 0.5
        decay = jnp.exp(-jnp.exp(log_w))
        a = jax.nn.sigmoid((p['rwkv_a0'][d] + a_dn[..., d * AAA_LORA:(d + 1) * AAA_LORA] @ p['rwkv_a_up'][d]).astype(F32))
        k_mod = k.astype(F32) * (1.0 + (a - 1.0) * p['rwkv_k_a'].astype(F32))
        per_dir.append((heads(decay), heads(k_mod), heads(a)))
    return heads(r), heads(k), heads(v), kk, g, per_dir


def rwkv7_scan(r, decay, k, v, kk, a, state0, reverse, with_outputs):
    def step(S, inp):
        r_t, w_t, k_t, v_t, kk_t, a_t = inp
        sa = jnp.einsum('bhvk,bhk->bhv', S, kk_t)
        S = S * w_t[:, :, None, :] - sa[..., None] * (kk_t * a_t)[:, :, None, :] + v_t[..., None] * k_t[:, :, None, :]
        y = jnp.einsum('bhvk,bhk->bhv', S, r_t) if with_outputs else None
        return S, y
    xs = tuple(jnp.moveaxis(t, 1, 0) for t in (r, decay, k, v, kk, a))
    S, ys = lax.scan(step, state0, xs, reverse=reverse)
    return S, (jnp.moveaxis(ys, 0, 1) if with_outputs else None)


def rwkv_output(y, prep, p):
    r, k, v, kk, g, _ = prep
    B, L, H, N = y.shape
    mu = jnp.mean(y, axis=-1, keepdims=True)
    var = jnp.mean(jnp.square(y - mu), axis=-1, keepdims=True)
    y = ((y - mu) * lax.rsqrt(var + RWKV_GN_EPS)).reshape(B, L, RWKV_DIM)
    y = y * p['rwkv_ln_w'].astype(F32) + p['rwkv_ln_b'].astype(F32)
    bonus = jnp.sum(r * k * p['rwkv_r_k'].astype(F32), axis=-1, keepdims=True) * v
    y = (y + bonus.reshape(B, L, RWKV_DIM)) * g.astype(F32)
    return y.astype(g.dtype)


def rwkv_mixer(prep_ctx, prep_lat, p, with_ctx):
    B = prep_lat[0].shape[0]
    zero = jnp.zeros((B, RWKV_HEADS, HEAD_DIM, HEAD_DIM), F32)

    def run(prep, d, S0, with_out):
        r, k, v, kk, g, per_dir = prep
        decay, k_mod, a = per_dir[d]
        return rwkv7_scan(r, decay, k_mod, v, kk, a, S0, d == 1, with_out)

    S_cf, y_cf = run(prep_ctx, 0, zero, with_ctx)
    S_cb, y_cb = run(prep_ctx, 1, zero, with_ctx)
    _, y_lf = run(prep_lat, 0, S_cf, True)
    _, y_lb = run(prep_lat, 1, S_cb, True)
    y_lat = rwkv_output(y_lf + y_lb, prep_lat, p)
    y_ctx = rwkv_output(y_cf + y_cb, prep_ctx, p) if with_ctx else None
    return y_ctx, y_lat


def mla_prepare(parts, p, rope):
    q_dn, kv_dn, k_rope = parts
    B, L, _ = q_dn.shape
    q = (rmsnorm(q_dn, p['mla_q_norm']) @ p['mla_w_uq']).reshape(B, L, MLA_HEADS, MLA_QK)
    kv = (rmsnorm(kv_dn, p['mla_kv_norm']) @ p['mla_w_ukv']).reshape(B, L, MLA_HEADS, MLA_NOPE + MLA_V)
    q = jnp.concatenate([q[..., :MLA_NOPE], apply_rope(q[..., MLA_NOPE:], rope)], axis=-1)
    k_rope = apply_rope(k_rope[:, :, None, :], rope)
    k = jnp.concatenate([kv[..., :MLA_NOPE], jnp.broadcast_to(k_rope, (B, L, MLA_HEADS, MLA_ROPE))], axis=-1)
    return q, k, kv[..., MLA_NOPE:]


def attend_blocks(q, k, v):
    B, Lq, H, dq = q.shape
    nb = Lq // Q_BLOCK
    qb = jnp.moveaxis(q.reshape(B, nb, Q_BLOCK, H, dq), 1, 0)
    scale = dq ** -0.5

    def one(q_blk):
        s = jnp.einsum('bqhd,bkhd->bhqk', q_blk, k, preferred_element_type=F32) * scale
        prob = jax.nn.softmax(s, axis=-1).astype(v.dtype)
        return jnp.einsum('bhqk,bkhd->bqhd', prob, v)

    out = lax.map(one, qb)
    return jnp.moveaxis(out, 0, 1).reshape(B, Lq, H * v.shape[-1])


def retention_prepare(parts, rope):
    q, k, v, g = parts
    B, L, _ = q.shape
    q = apply_rope(q.reshape(B, L, RET_HEADS, RET_KEY), rope).astype(F32)
    k = apply_rope(k.reshape(B, L, RET_HEADS, RET_KEY), rope).astype(F32) * (RET_KEY ** -0.5)
    v = v.reshape(B, L, RET_HEADS, RET_VAL).astype(F32)
    return q, k, v, g


def retention_chunkwise(q, k, v, log_gamma, R0, with_outputs):
    B, L, H, dk = q.shape
    dv = v.shape[-1]
    C = RET_CHUNK
    n = L // C
    idx = jnp.arange(C, dtype=F32)
    rel = idx[:, None] - idx[None, :]
    inner = jnp.where(rel[None] >= 0, jnp.exp(log_gamma[:, None, None] * jnp.maximum(rel, 0.0)[None]), 0.0)
    cross = jnp.exp(log_gamma[None, :] * (idx[:, None] + 1.0))
    tail = jnp.exp(log_gamma[None, :] * (C - 1.0 - idx)[:, None])
    chunk_decay = jnp.exp(log_gamma * C)
    to_chunks = lambda t: jnp.moveaxis(t.reshape(B, n, C, H, t.shape[-1]), 1, 0)

    def step(R, inp):
        qc, kc, vc = inp
        R_new = R * chunk_decay[None, :, None, None] + jnp.einsum('bjhd,jh,bjhe->bhde', kc, tail, vc)
        if with_outputs:
            s = jnp.einsum('bihd,bjhd->bhij', qc, kc) * inner[None]
            o = jnp.einsum('bhij,bjhe->bihe', s, vc) + jnp.einsum('bihd,bhde->bihe', qc, R) * cross[None, :, :, None]
        else:
            o = None
        return R_new, o

    R, o = lax.scan(step, R0, (to_chunks(q), to_chunks(k), to_chunks(v)))
    if with_outputs:
        o = jnp.moveaxis(o, 0, 1).reshape(B, L, H, dv)
    return R, o


def retention_output(y, g):
    B, L = y.shape[0], y.shape[1]
    y = y * lax.rsqrt(jnp.mean(y * y, axis=-1, keepdims=True) + RMS_EPS)
    return (jax.nn.silu(g.astype(F32)) * y.reshape(B, L, RET_DIM)).astype(g.dtype)


def retention_mixer(prep_ctx, prep_lat, p, with_ctx):
    B = prep_lat[0].shape[0]
    zero = jnp.zeros((B, RET_HEADS, RET_KEY, RET_VAL), F32)
    log_gamma = jax.nn.log_sigmoid(p['ret_decay'].astype(F32))
    flip = lambda t: jnp.flip(t, axis=1)

    def run(prep, d, R0, with_out):
        q, k, v, _ = prep
        if d == 1:
            q, k, v = flip(q), flip(k), flip(v)
        R, o = retention_chunkwise(q, k, v, log_gamma[d], R0, with_out)
        if d == 1 and with_out:
            o = flip(o)
        return R, o

    R_cf, o_cf = run(prep_ctx, 0, zero, with_ctx)
    R_cb, o_cb = run(prep_ctx, 1, zero, with_ctx)
    _, o_lf = run(prep_lat, 0, R_cf, True)
    _, o_lb = run(prep_lat, 1, R_cb, True)
    y_lat = retention_output(o_lf + o_lb, prep_lat[3])
    y_ctx = retention_output(o_cf + o_cb, prep_ctx[3]) if with_ctx else None
    return y_ctx, y_lat


def merge_branches(h, y_a, y_b, y_c, p):
    gates = jax.nn.sigmoid((h @ p['w_branch_gate'] + p['b_branch_gate']).astype(F32)).astype(h.dtype)
    g_a, g_b, g_c = jnp.split(gates, 3, axis=-1)
    m = g_a * (y_a @ p['w_branch_a']) + g_b * (y_b @ p['w_branch_b']) + g_c * (y_c @ p['w_branch_c'])
    return m @ p['w_out']


def hier_moe(h, p):
    n_tok = h.shape[0]
    grp_prob = jax.nn.softmax((h @ p['moe_w_group']).astype(F32) + p['moe_b_group'].astype(F32), axis=-1)
    grp_p, grp_idx = lax.top_k(grp_prob, 1)
    exp_logits = ((h @ p['moe_w_expert']).astype(F32) + p['moe_b_expert'].astype(F32)).reshape(n_tok, N_GROUPS, EXPERTS_PER_GROUP)
    in_group = jnp.einsum('nge,ng->ne', exp_logits, jax.nn.one_hot(grp_idx[:, 0], N_GROUPS, dtype=F32))
    exp_p, exp_idx = lax.top_k(jax.nn.softmax(in_group, axis=-1), TOP_K)
    weights = grp_p * exp_p / jnp.sum(exp_p, axis=-1, keepdims=True)
    expert_id = grp_idx * EXPERTS_PER_GROUP + exp_idx
    combine = jnp.einsum('nk,nke->ne', weights, jax.nn.one_hot(expert_id, N_EXPERTS, dtype=F32)).astype(h.dtype)
    y = jnp.zeros_like(h)
    for e in range(N_EXPERTS):
        hid = jax.nn.silu(h @ p['moe_w_gate'][e]) * (h @ p['moe_w_up'][e])
        y = y + (hid @ p['moe_w_down'][e]) * combine[:, e:e + 1]
    return y


def hybrid_layer(x_ctx, x_lat, mod_ctx, mod_lat, p, ropes, with_ctx):
    sh1c, sc1c, g1c, sh2c, sc2c, g2c = mod_ctx
    sh1l, sc1l, g1l, sh2l, sc2l, g2l = mod_lat
    h_ctx = rmsnorm(x_ctx, p['norm1_w']) * (1 + sc1c) + sh1c
    h_lat = rmsnorm(x_lat, p['norm1_w']) * (1 + sc1l) + sh1l
    pc = split_columns(h_ctx @ p['w_in'])
    pl = split_columns(h_lat @ p['w_in'])
    ya_ctx, ya_lat = rwkv_mixer(rwkv_prepare(pc[0:6], p), rwkv_prepare(pl[0:6], p), p, with_ctx)
    q_c, k_c, v_c = mla_prepare(pc[6:9], p, None)
    q_l, k_l, v_l = mla_prepare(pl[6:9], p, ropes[0])
    yb_lat = attend_blocks(q_l, jnp.concatenate([k_c, k_l], axis=1), jnp.concatenate([v_c, v_l], axis=1))
    yc_ctx, yc_lat = retention_mixer(retention_prepare(pc[9:13], None), retention_prepare(pl[9:13], ropes[1]), p, with_ctx)
    x_lat = x_lat + g1l * merge_branches(h_lat, ya_lat, yb_lat, yc_lat, p)
    h2_lat = rmsnorm(x_lat, p['norm2_w']) * (1 + sc2l) + sh2l
    if with_ctx:
        yb_ctx = attend_blocks(q_c, k_c, v_c)
        x_ctx = x_ctx + g1c * merge_branches(h_ctx, ya_ctx, yb_ctx, yc_ctx, p)
        h2_ctx = rmsnorm(x_ctx, p['norm2_w']) * (1 + sc2c) + sh2c
        n_ctx = h2_ctx.shape[0] * h2_ctx.shape[1]
        tokens = jnp.concatenate([h2_ctx.reshape(-1, D_MODEL), h2_lat.reshape(-1, D_MODEL)], axis=0)
        f = hier_moe(tokens, p)
        x_ctx = x_ctx + g2c * f[:n_ctx].reshape(x_ctx.shape)
        x_lat = x_lat + g2l * f[n_ctx:].reshape(x_lat.shape)
    else:
        x_lat = x_lat + g2l * hier_moe(h2_lat.reshape(-1, D_MODEL), p).reshape(x_lat.shape)
    return x_ctx, x_lat


def setup_inputs(seed: int = 0) -> dict:
    key = jax.random.key(seed)
    ks = iter(jax.random.split(key, 48))
    nrm = lambda shape, scale: jax.random.normal(next(ks), shape, F32) * scale
    D = D_MODEL
    head_idx = jnp.arange(RET_HEADS, dtype=F32)
    ret_logit = jnp.log(2.0 ** (5.0 + head_idx) - 1.0)
    return {
        'x': nrm((BATCH, SEQ, D), 1.0),
        'c': nrm((BATCH, D), 1.0),
        'ctx': nrm((BATCH, CTX_LEN, D), 1.0),
        'c_ctx': nrm((D,), 1.0),
        'w_mod': nrm((DEPTH, D, N_MOD * D), 0.5 * D ** -0.5),
        'b_mod': nrm((DEPTH, N_MOD * D), 0.02),
        'norm1_w': 1.0 + nrm((DEPTH, D), 0.02),
        'w_in': nrm((DEPTH, D, D_IN), D ** -0.5),
        'rwkv_conv': nrm((DEPTH, 3, 3 * RWKV_DIM), 0.3) + jax.nn.one_hot(1, 3, dtype=F32)[None, :, None],
        'rwkv_w0': jax.random.uniform(next(ks), (DEPTH, 2, RWKV_DIM), F32, -6.0, 1.0),
        'rwkv_w_up': nrm((DEPTH, 2, DECAY_LORA, RWKV_DIM), 0.5 * DECAY_LORA ** -0.5),
        'rwkv_a0': nrm((DEPTH, 2, RWKV_DIM), 0.5),
        'rwkv_a_up': nrm((DEPTH, 2, AAA_LORA, RWKV_DIM), 0.5 * AAA_LORA ** -0.5),
        'rwkv_g_up': nrm((DEPTH, GATE_LORA, RWKV_DIM), GATE_LORA ** -0.5),
        'rwkv_k_k': 0.85 + nrm((DEPTH, RWKV_DIM), 0.05),
        'rwkv_k_a': 1.0 + nrm((DEPTH, RWKV_DIM), 0.05),
        'rwkv_r_k': nrm((DEPTH, RWKV_HEADS, HEAD_DIM), 0.1),
        'rwkv_ln_w': 1.0 + nrm((DEPTH, RWKV_DIM), 0.02),
        'rwkv_ln_b': nrm((DEPTH, RWKV_DIM), 0.02),
        'mla_q_norm': 1.0 + nrm((DEPTH, MLA_Q_RANK), 0.02),
        'mla_w_uq': nrm((DEPTH, MLA_Q_RANK, MLA_HEADS * MLA_QK), MLA_Q_RANK ** -0.5),
        'mla_kv_norm': 1.0 + nrm((DEPTH, MLA_KV_RANK), 0.02),
        'mla_w_ukv': nrm((DEPTH, MLA_KV_RANK, MLA_HEADS * (MLA_NOPE + MLA_V)), MLA_KV_RANK ** -0.5),
        'ret_decay': ret_logit + nrm((DEPTH, 2, RET_HEADS), 0.1),
        'w_branch_a': nrm((DEPTH, RWKV_DIM, D), RWKV_DIM ** -0.5),
        'w_branch_b': nrm((DEPTH, MLA_DIM, D), MLA_DIM ** -0.5),
        'w_branch_c': nrm((DEPTH, RET_DIM, D), RET_DIM ** -0.5),
        'w_branch_gate': nrm((DEPTH, D, 3 * D), D ** -0.5),
        'b_branch_gate': nrm((DEPTH, 3 * D), 0.02),
        'w_out': nrm((DEPTH, D, D), D ** -0.5),
        'norm2_w': 1.0 + nrm((DEPTH, D), 0.02),
        'moe_w_group': nrm((DEPTH, D, N_GROUPS), D ** -0.5),
        'moe_b_group': nrm((DEPTH, N_GROUPS), 0.01),
        'moe_w_expert': nrm((DEPTH, D, N_EXPERTS), D ** -0.5),
        'moe_b_expert': nrm((DEPTH, N_EXPERTS), 0.01),
        'moe_w_gate': nrm((DEPTH, N_EXPERTS, D, EXPERT_HIDDEN), D ** -0.5),
        'moe_w_up': nrm((DEPTH, N_EXPERTS, D, EXPERT_HIDDEN), D ** -0.5),
        'moe_w_down': nrm((DEPTH, N_EXPERTS, EXPERT_HIDDEN, D), EXPERT_HIDDEN ** -0.5),
        'final_norm_w': 1.0 + nrm((D,), 0.02),
    }


def reference(x, c, ctx, c_ctx, w_mod, b_mod, norm1_w, w_in, rwkv_conv, rwkv_w0, rwkv_w_up, rwkv_a0,
              rwkv_a_up, rwkv_g_up, rwkv_k_k, rwkv_k_a, rwkv_r_k, rwkv_ln_w, rwkv_ln_b, mla_q_norm,
              mla_w_uq, mla_kv_norm, mla_w_ukv, ret_decay, w_branch_a, w_branch_b, w_branch_c,
              w_branch_gate, b_branch_gate, w_out, norm2_w, moe_w_group, moe_b_group, moe_w_expert,
              moe_b_expert, moe_w_gate, moe_w_up, moe_w_down, final_norm_w):
    n_lat = x.shape[1]
    ropes = (axial_rope_table(n_lat, MLA_ROPE), axial_rope_table(n_lat, RET_KEY))
    x_ctx, x_lat = ctx, x
    for l in range(DEPTH):
        p = {
            'norm1_w': norm1_w[l], 'w_in': w_in[l], 'rwkv_conv': rwkv_conv[l], 'rwkv_w0': rwkv_w0[l],
            'rwkv_w_up': rwkv_w_up[l], 'rwkv_a0': rwkv_a0[l], 'rwkv_a_up': rwkv_a_up[l],
            'rwkv_g_up': rwkv_g_up[l], 'rwkv_k_k': rwkv_k_k[l], 'rwkv_k_a': rwkv_k_a[l],
            'rwkv_r_k': rwkv_r_k[l], 'rwkv_ln_w': rwkv_ln_w[l], 'rwkv_ln_b': rwkv_ln_b[l],
            'mla_q_norm': mla_q_norm[l], 'mla_w_uq': mla_w_uq[l], 'mla_kv_norm': mla_kv_norm[l],
            'mla_w_ukv': mla_w_ukv[l], 'ret_decay': ret_decay[l], 'w_branch_a': w_branch_a[l],
            'w_branch_b': w_branch_b[l], 'w_branch_c': w_branch_c[l], 'w_branch_gate': w_branch_gate[l],
            'b_branch_gate': b_branch_gate[l], 'w_out': w_out[l], 'norm2_w': norm2_w[l],
            'moe_w_group': moe_w_group[l], 'moe_b_group': moe_b_group[l],
            'moe_w_expert': moe_w_expert[l], 'moe_b_expert': moe_b_expert[l],
            'moe_w_gate': moe_w_gate[l], 'moe_w_up': moe_w_up[l], 'moe_w_down': moe_w_down[l],
        }
        mod_lat = jnp.split((jax.nn.silu(c) @ w_mod[l] + b_mod[l])[:, None, :], N_MOD, axis=-1)
        mod_ctx = jnp.split((jax.nn.silu(c_ctx) @ w_mod[l] + b_mod[l])[None, None, :], N_MOD, axis=-1)
        x_ctx, x_lat = hybrid_layer(x_ctx, x_lat, mod_ctx, mod_lat, p, ropes, l < DEPTH - 1)
    return rmsnorm(x_lat, final_norm_w)
```

```python
import numpy as np
import ml_dtypes
import concourse.bass as bass
import concourse.mybir as mybir
from concourse.bass_utils import run_bass_kernel_spmd
from contextlib import ExitStack
import os

F32 = mybir.dt.float32
BF16 = mybir.dt.bfloat16
AF = mybir.ActivationFunctionType
ALU = mybir.AluOpType
AX = mybir.AxisListType
ENG_ATTR = {"pe": "tensor", "act": "scalar", "dve": "vector", "pool": "gpsimd", "sp": "sync"}

T = 4352
NT = 34
D = 1024
DIN = 2592
NCTX = 256
BLK = [(0, 256)] + [(256 + 512 * i, 512) for i in range(8)]
EPS = 1e-6


class Prog:
    def __init__(self, nc, stack, n_dma_sems=6, same_engine_sync=True):
        self.nc = nc
        self.stack = stack
        self.ops = []
        self.lastw = {}
        self.readers = {}
        self.n_dma_sems = n_dma_sems
        self.same_engine_sync = same_engine_sync
        self._n = 0
        self.last_of = {}
        self.dmas_since = []

    def sb(self, shape, dtype, name=None):
        self._n += 1
        return self.stack.enter_context(self.nc.sbuf_tensor(name or f"sb{self._n}", list(shape), dtype))

    def ps(self, shape, dtype, name=None):
        self._n += 1
        return self.stack.enter_context(self.nc.psum_tensor(name or f"ps{self._n}", list(shape), dtype))

    def op(self, eng, fn, r=(), w=(), dma=False, extra=()):
        idx = len(self.ops)
        deps = set(extra)
        w = list(w) + [k for k in r if k.startswith("BK")]
        r = [k for k in r if not k.startswith("BK")]
        for k in r:
            if k in self.lastw:
                deps.add(self.lastw[k])
        for k in w:
            if k in self.lastw:
                deps.add(self.lastw[k])
            rd = self.readers.get(k)
            if rd:
                for kk, v in rd.items():
                    if kk == "dma":
                        deps.update(v)
                    else:
                        deps.add(v)
        for k in r:
            rd = self.readers.setdefault(k, {})
            if dma:
                rd.setdefault("dma", []).append(idx)
            else:
                rd[eng] = idx
        for k in w:
            self.lastw[k] = idx
            self.readers[k] = {}
        deps.discard(idx)
        self.ops.append((eng, fn, deps, dma))
        if dma:
            self.dmas_since.append(idx)
        else:
            self.last_of[eng] = idx
        return idx

    def dma(self, eng, out, in_, r=(), w=(), **kw):
        return self.op(eng, lambda e: e.dma_start(out=out, in_=in_, **kw), r, w, dma=True)

    def barrier(self):
        deps = set(self.last_of.values()) | set(self.dmas_since)
        for e in ["pe", "act", "dve", "pool", "sp"]:
            self.op(e, None, extra=deps)
        self.lastw = {}
        self.readers = {}
        self.dmas_since = []

    def emit(self):
        nc = self.nc
        ops = self.ops
        NEP = 4
        needed = set()
        for (eng, fn, deps, dma) in ops:
            for d in deps:
                deng, dfn, _, ddma = ops[d]
                if dfn is None:
                    continue
                if deng == eng and not ddma and (eng == "pe" or not self.same_engine_sync):
                    continue
                needed.add(d)
        engs = ["pe", "act", "dve", "pool", "sp"]
        esem = {e: [self.stack.enter_context(nc.semaphore(f"s_{e}{j}")) for j in range(NEP)] for e in engs}
        nds = {"sp": self.n_dma_sems, "pool": int(os.environ.get("POOLSEMS", "6")), "act": 2}
        dsem = {e: [self.stack.enter_context(nc.semaphore(f"d_{e}{i}")) for i in range(nds[e])]
                for e in ["sp", "pool", "act"]}
        ecnt = {e: [0] * NEP for e in engs}
        epoch = {e: 0 for e in engs}
        dcnt = {e: [0] * nds[e] for e in dsem}
        dnum = {e: 0 for e in dsem}
        val = {}
        prev = {}
        for i, (eng, fn, deps, dma) in enumerate(ops):
            if fn is None:
                epoch[eng] += 1
                continue
            if dma:
                j = dnum[eng] % nds[eng]
                dnum[eng] += 1
                prev[i] = (dsem[eng][j], dcnt[eng][j])
                dcnt[eng][j] += 16
                val[i] = (dsem[eng][j], dcnt[eng][j])
            elif i in needed:
                j = epoch[eng] % NEP
                ecnt[eng][j] += 1
                val[i] = (esem[eng][j], ecnt[eng][j])
        per = {e: [] for e in engs}
        for i, o in enumerate(ops):
            per[o[0]].append(i)
        self.stats = {e: len(per[e]) for e in engs}
        self.stats['ecnt'] = {e: max(v) for e, v in ecnt.items()}
        self.stats['dcnt'] = {k: max(v) for k, v in dcnt.items()}
        with nc.Block() as block:
            for e in engs:
                def body(engine, e=e):
                    waited = {}

                    def wait(sem, v):
                        if v <= 0:
                            return
                        key = id(sem)
                        if waited.get(key, 0) >= v:
                            return
                        engine.wait_ge(sem, v)
                        waited[key] = v
                    for i in per[e]:
                        eng, fn, deps, dma = ops[i]
                        for d in sorted(deps):
                            if d in val:
                                wait(*val[d])
                        if fn is None:
                            continue
                        if dma:
                            wait(*prev[i])
                        ins = fn(engine)
                        if dma:
                            ins.then_inc(val[i][0], 16)
                        elif i in val:
                            ins.then_inc(val[i][0], 1)
                    if e == "sp":
                        for q in dsem:
                            for j in range(nds[q]):
                                wait(dsem[q][j], dcnt[q][j])
                        for q in engs:
                            if q != "sp":
                                for j in range(NEP):
                                    wait(esem[q][j], ecnt[q][j])
                getattr(block, ENG_ATTR[e])(body)


class Arena:
    def __init__(self, P, words):
        self.t = P.sb([128, words], F32, "arena")
        self.words = words
        self.off = 0

    def mark(self):
        return self.off

    def release(self, m):
        self.off = m

    def f32(self, n):
        assert self.off + n <= self.words, ("arena overflow", self.off, n)
        ap = self.t[:, self.off:self.off + n]
        self.off += n
        return ap

    def bf16(self, n):
        w = (n + 1) // 2
        assert self.off + w <= self.words, ("arena overflow", self.off, w)
        ap = self.t[:, self.off:self.off + w].bitcast(BF16)
        self.off += w
        return ap[:, 0:n]


def MM(P, out, lhsT, rhs, start, stop, r, w):
    P.op("pe", lambda e: e.matmul(out, lhsT=lhsT, rhs=rhs, start=start, stop=stop), r, w)


def TR(P, out, in_, ident, r, w):
    P.op("pe", lambda e: e.transpose(out, in_, ident), r, w)


def ACT(P, out, in_, func, r, w, **kw):
    P.op("act", lambda e: e.activation(out=out, in_=in_, func=func, **kw), r, w)


def TT(P, eng, out, in0, in1, op, r, w):
    P.op(eng, lambda e: e.tensor_tensor(out=out, in0=in0, in1=in1, op=op), r, w)


def TS(P, eng, out, in0, s1, s2, op0, op1, r, w, **kw):
    if op1 is None:
        P.op(eng, lambda e: e.tensor_scalar(out=out, in0=in0, scalar1=s1, scalar2=None, op0=op0, **kw), r, w)
    else:
        P.op(eng, lambda e: e.tensor_scalar(out=out, in0=in0, scalar1=s1, scalar2=s2, op0=op0, op1=op1, **kw), r, w)


def STT(P, out, in0, scalar, in1, op0, op1, r, w):
    P.op("dve", lambda e: e.scalar_tensor_tensor(out=out, in0=in0, scalar=scalar, in1=in1, op0=op0, op1=op1), r, w)


def CP(P, eng, out, in_, r, w):
    if eng == "act":
        P.op("act", lambda e: e.activation(out=out, in_=in_, func=AF.Identity), r, w)
    else:
        P.op(eng, lambda e: e.tensor_copy(out=out, in_=in_), r, w)


def RED(P, out, in_, op, r, w, axis=AX.X):
    P.op("dve", lambda e: e.tensor_reduce(out=out, in_=in_, axis=axis, op=op), r, w)


def RECIP(P, out, in_, r, w):
    P.op("dve", lambda e: e.reciprocal(out=out, in_=in_), r, w)


def MEMSET(P, eng, ap, v, r, w):
    P.op(eng, lambda e: e.memset(ap, v), r, w)


def b3(ap, n, axis):
    p, m = ap.shape[0], ap.shape[1]
    if axis == 1:
        return ap.unsqueeze(1).broadcast_to([p, n, m])
    return ap.unsqueeze(2).broadcast_to([p, m, n])


def stage_init(K, n=2, words=4096):
    K.stg = [K.A.f32(words) for _ in range(n)]
    K.stg_words = words
    K.stg_i = getattr(K, "stg_i", 0)


def load_cast(K, dst, src, key, q="sp", cast_eng="pool"):
    P = K.P
    if len(dst.shape) == 2:
        dst = dst.unsqueeze(1)
        src = src.unsqueeze(1)
    p, a, b = dst.shape
    assert b <= K.stg_words, b
    step = max(1, K.stg_words // b)
    for a0 in range(0, a, step):
        a1 = min(a, a0 + step)
        i = K.stg_i % len(K.stg)
        K.stg_i += 1
        st = K.stg[i][0:p, 0:(a1 - a0) * b].rearrange("p (a b) -> p a b", b=b)
        if hasattr(K.stg[i], "shape") and K.stg[i].shape[0] != p:
            pass
        P.dma(q, st, src[:, a0:a1, :], w=[f"stg{i}"])
        P.op(cast_eng, lambda e, o=dst[:, a0:a1, :], x=st: e.tensor_copy(out=o, in_=x), r=[f"stg{i}"], w=[key])


class Ctx:
    pass


WSHAPES = {
    'w_mod': (2, D, 6 * D), 'b_mod': (2, 6 * D), 'norm1_w': (2, D), 'w_in': (2, D, DIN),
    'rwkv_conv': (2, 3, 768), 'rwkv_w0': (2, 2, 256), 'rwkv_w_up': (2, 2, 64, 256), 'rwkv_a0': (2, 2, 256),
    'rwkv_a_up': (2, 2, 64, 256), 'rwkv_g_up': (2, 128, 256), 'rwkv_k_k': (2, 256), 'rwkv_k_a': (2, 256),
    'rwkv_r_k': (2, 4, 64), 'rwkv_ln_w': (2, 256), 'rwkv_ln_b': (2, 256), 'mla_q_norm': (2, 256),
    'mla_w_uq': (2, 256, 768), 'mla_kv_norm': (2, 128), 'mla_w_ukv': (2, 128, 1024), 'ret_decay': (2, 2, 4),
    'w_branch_a': (2, 256, D), 'w_branch_b': (2, 512, D), 'w_branch_c': (2, 256, D),
    'w_branch_gate': (2, D, 3 * D), 'b_branch_gate': (2, 3 * D), 'w_out': (2, D, D), 'norm2_w': (2, D),
    'moe_w_group': (2, D, 4), 'moe_b_group': (2, 4), 'moe_w_expert': (2, D, 32), 'moe_b_expert': (2, 32),
    'moe_w_gate': (2, 32, D, 512), 'moe_w_up': (2, 32, D, 512), 'moe_w_down': (2, 32, 512, D),
    'final_norm_w': (D,),
}


def make_consts():
    c = {}
    c['ident_f'] = np.eye(128, dtype=np.float32)
    c['ident_b'] = np.eye(128).astype(ml_dtypes.bfloat16)
    c['ones_f'] = np.ones((128, 128), np.float32)

    def rope_tab(rot_dim):
        n = 4096
        row = np.repeat(np.arange(n // 64, dtype=np.float32), 64)
        col = np.tile(np.arange(64, dtype=np.float32), n // 64)
        nf = rot_dim // 4
        inv = (np.float32(10000.0) ** (-np.arange(nf, dtype=np.float32) / nf)).astype(np.float32)
        ang = np.concatenate([row[:, None] * inv, col[:, None] * inv], axis=-1).astype(np.float32)
        cos, sin = np.cos(ang).astype(np.float32), np.sin(ang).astype(np.float32)
        return np.concatenate([cos, cos], 1), np.concatenate([-sin, sin], 1)
    c['rope_m_c'], c['rope_m_s'] = rope_tab(32)
    c['rope_r_c'], c['rope_r_s'] = rope_tab(64)
    c['rope_rk_c'], c['rope_rk_s'] = c['rope_r_c'] * np.float32(0.125), c['rope_r_s'] * np.float32(0.125)
    jj = np.arange(128, dtype=np.float32)[:, None]
    ii = np.arange(128, dtype=np.float32)[None, :]
    c['ret_rel'] = np.stack([np.maximum(ii - jj, 0), (ii >= jj).astype(np.float32),
                             np.maximum(jj - ii, 0), (jj >= ii).astype(np.float32)], 1).astype(np.float32)
    pp = np.arange(128)
    same = (pp[:, None] // 32) == (pp[None, :] // 32)
    cm, mka, mkc = [], [], []
    for d in range(2):
        jj_, ss_ = pp[:, None], pp[None, :]
        before = same & ((jj_ < ss_) if d == 0 else (jj_ > ss_))
        beq = before | (jj_ == ss_)
        after = same & ~beq
        f = lambda m: m.astype(np.float32)
        cm.append(np.stack([-f(before), f(beq), -f(beq), -f(after)], 1))
        mka.append(np.stack([f(before), f(beq), f(before), f(beq)], 1))
        mkc.append(f(before).T.copy())
    c['rw_cm'] = np.stack(cm).astype(np.float32)
    c['rw_mka'] = np.stack(mka).astype(np.float32)
    c['rw_mkc'] = np.stack(mkc).astype(np.float32)
    hm = (pp[:, None] // 32 == np.arange(4)[None, :]).astype(np.float32)
    c['rw_hm'] = np.concatenate([hm, -hm], 1)
    c['identb'] = np.tile(np.eye(64, dtype=np.float32)[:, None, :], (1, 4, 1))
    p = np.arange(128, dtype=np.float32)
    c['ret_pos'] = np.stack([p + 1, 128 - p, 127 - p, p], 1).astype(np.float32)
    return c


def build(n_layers=2, upto=99, debug=()):
    nc = bass.Bass("TRN2", target_bir_lowering=False)
    K = Ctx()
    K.nc = nc
    K.debug = set(debug)
    K.n_layers = n_layers
    K.upto = upto
    consts = make_consts()
    K.consts = consts
    K.I = {}
    K.I['xin'] = nc.dram_tensor("xin", [T, D], F32, kind="ExternalInput").ap()
    K.I['cc'] = nc.dram_tensor("cc", [2, D], F32, kind="ExternalInput").ap()
    for n, s in WSHAPES.items():
        K.I[n] = nc.dram_tensor(n, list(s), F32, kind="ExternalInput").ap()
    for n, a in consts.items():
        dt = BF16 if a.dtype == ml_dtypes.bfloat16 else F32
        K.I[n] = nc.dram_tensor(n, list(a.shape), dt, kind="ExternalInput").ap()
    K.out = nc.dram_tensor("out", [4096, D], F32, kind="ExternalOutput").ap()

    def scratch(name, shape, dt):
        kind = "ExternalOutput" if name in K.debug else "Internal"
        return nc.dram_tensor(name, list(shape), dt, kind=kind).ap()
    K.S = {}
    K.S['u'] = scratch("u", [T, DIN], F32)
    K.S['gatesT'] = scratch("gatesT", [24, 128, T], BF16)
    K.S['modrow'] = scratch("modrow", [2, 2, 8, 128], F32)
    K.S['QT'] = scratch("QT", [8, 96, T], BF16)
    K.S['KT'] = scratch("KT", [8, 96, T], BF16)
    K.S['ybT'] = scratch("ybT", [8, 64, T], BF16)
    K.S['ycT'] = scratch("ycT", [2, 128, T], BF16)
    K.S['yaT'] = scratch("yaT", [2, 128, T], BF16)
    K.S['rwp0'] = scratch("rwp0", [4, T, 512], F32)
    K.S['rwp1'] = scratch("rwp1", [4, T, 512], F32)
    K.S['gb'] = scratch("gb", [T, 260], F32)
    K.S['yd0'] = scratch("yd0", [T, 256], F32)
    K.S['yd1'] = scratch("yd1", [T, 256], F32)
    K.S['xmid'] = scratch("xmid", [T, D], F32)
    K.S['xnext'] = scratch("xnext", [T, D], F32)
    K.S['h2T'] = scratch("h2T", [8, 128, T], BF16)
    K.S['comb'] = scratch("comb", [T, 32], F32)

    with ExitStack() as st:
        P = Prog(nc, st)
        K.P = P
        K.A = Arena(P, 52000)
        K.banks = [P.ps([128, 512], F32, f"bank{i}") for i in range(8)]
        K.ident_f = P.sb([128, 128], F32, "ident_f_sb")
        K.ident_b = P.sb([128, 128], BF16, "ident_b_sb")
        K.ones_f = P.sb([128, 128], F32, "ones_f_sb")
        K.MOD = P.sb([128, 48, 2], F32, "MOD")
        K.G1 = P.sb([128, 8, 2], F32, "G1")
        K.G2 = P.sb([128, 8, 2], F32, "G2")
        K.bg = P.sb([128, 24], F32, "bg")
        P.dma("sp", K.ident_f[:], K.I['ident_f'][:, :], w=["ident_f"])
        P.dma("sp", K.ident_b[:], K.I['ident_b'][:, :], w=["ident_b"])
        P.dma("sp", K.ones_f[:], K.I['ones_f'][:, :], w=["ones_f"])
        P.barrier()
        for l in range(n_layers):
            K.l = l
            K.with_ctx = l < n_layers - 1 or n_layers == 1 and K.upto < 99
            K.xcur = K.I['xin'] if l == 0 else K.S['xnext']
            run_layer(K)
        P.barrier()
        P.emit()
        K.stats = P.stats
    return nc, K


def run_layer(K):
    phases = [phase_mod, phase_inproj, phase_mla_prep, phase_attn, phase_ret, phase_rwkv_prep, phase_rwkv_scan, phase_rwkv_out,
              phase_merge, phase_moe]
    import os
    sel = os.environ.get("PHASES")
    if sel:
        phases = [phases[int(x)] for x in sel.split(",")]
    for i, ph in enumerate(phases):
        if i > K.upto:
            break
        ph(K)
        K.P.barrier()


def bank_bf(K, i):
    return K.banks[i][:, :].bitcast(BF16)


def phase_mod(K):
    P, A, I, l = K.P, K.A, K.I, K.l
    m0 = A.mark()
    RW = A.f32(128)
    COL = A.f32(104)
    wm = A.f32(8 * 3072).rearrange("p (k c) -> p k c", k=8)
    P.dma("sp", RW[0:16, :], I['cc'].rearrange("a (k p) -> (a k) p", p=128), w=["RW"])
    P.dma("sp", RW[16:64, :], I['b_mod'][l].rearrange("(j p) -> j p", p=128), w=["RW"])
    P.dma("sp", RW[64:72, :], I['norm1_w'][l].rearrange("(j p) -> j p", p=128), w=["RW"])
    P.dma("sp", RW[72:80, :], I['norm2_w'][l].rearrange("(j p) -> j p", p=128), w=["RW"])
    P.dma("sp", RW[80:104, :], I['b_branch_gate'][l].rearrange("(j p) -> j p", p=128), w=["RW"])
    b0, b1, b2 = K.banks[0], K.banks[1], K.banks[2]
    TR(P, b0[:, 0:104], RW[0:104, :], K.ident_f[0:104, 0:104], r=["RW", "ident_f"], w=["BK0"])
    ACT(P, COL[:, 0:16], b0[:, 0:16], AF.Silu, r=["BK0"], w=["COLa"])
    CP(P, "dve", COL[:, 16:104], b0[:, 16:104], r=["BK0"], w=["COLb"])
    CP(P, "dve", K.bg[:, :], b0[:, 80:104], r=["BK0"], w=["bg"])
    scr = COL[:, 0:16].rearrange("p (a k) -> p k a", a=2)
    for hf in range(2):
        for kc in range(8):
            P.dma("sp" if kc % 2 == 0 else "act", wm[:, kc, :],
                  I['w_mod'][l, kc * 128:(kc + 1) * 128, hf * 3072:(hf + 1) * 3072], w=[f"wm{kc}"])
        for j in range(24):
            jj = hf * 24 + j
            for kc in range(8):
                MM(P, b1[:, 2 * jj:2 * jj + 2], wm[:, kc, j * 128:(j + 1) * 128], scr[:, kc, :],
                   kc == 0, kc == 7, r=[f"wm{kc}", "COLa"], w=["BK1"])
    TT(P, "dve", K.MOD[:, :, :], b1[:, 0:96].rearrange("p (j a) -> p j a", a=2),
       b3(COL[:, 16:64], 2, 2), ALU.add, r=["BK1", "COLb"], w=["MOD"])
    STT(P, K.G1[:, :, :], K.MOD[:, 8:16, :], 1.0, b3(COL[:, 64:72], 2, 2), ALU.add, ALU.mult,
        r=["MOD", "COLb"], w=["G1"])
    STT(P, K.G2[:, :, :], K.MOD[:, 32:40, :], 1.0, b3(COL[:, 72:80], 2, 2), ALU.add, ALU.mult,
        r=["MOD", "COLb"], w=["G2"])
    GR = A.f32(32).rearrange("p (m a f) -> p m a f", m=2, a=2)
    GRr = A.f32(128)
    for mi, m in enumerate((2, 5)):
        CP(P, "dve", GR[:, mi, :, :], K.MOD[:, m * 8:(m + 1) * 8, :].rearrange("p f a -> p a f"),
           r=["MOD"], w=["GR"])
    TR(P, b2[0:32, 0:128], A.t[:, 0:0] if False else GR.rearrange("p m a f -> p (m a f)"), K.ident_f[:, :],
       r=["GR", "ident_f"], w=["BK2"])
    CP(P, "dve", GRr[0:32, :], b2[0:32, 0:128], r=["BK2"], w=["GRr"])
    P.dma("sp", K.S['modrow'].rearrange("m a f p -> (m a f) p"), GRr[0:32, :], r=["GRr"], w=["modrow"])
    A.release(m0)


def norm_tile(K, xt, xn, st, key):
    P = K.P
    ACT(P, xn, xt, AF.Square, r=[key + "x"], w=[key + "n", key + "s"], accum_out=st[:, 0:1])
    ACT(P, st[:, 1:2], st[:, 0:1], AF.Sqrt, r=[key + "s"], w=[key + "s1"], scale=1.0 / D, bias=EPS)
    RECIP(P, st[:, 2:3], st[:, 1:2], r=[key + "s1"], w=[key + "s2"])
    ACT(P, xn, xt, AF.Identity, r=[key + "x", key + "s2"], w=[key + "n"], scale=st[:, 2:3])


def phase_inproj(K):
    P, A, I, S, l = K.P, K.A, K.I, K.S, K.l
    m0 = A.mark()
    hT = A.bf16(8 * T).rearrange("p (k t) -> p k t", k=8)
    m1 = A.mark()
    wi = A.bf16(8 * DIN).rearrange("p (k c) -> p k c", k=8)
    stage_init(K)
    for kc in range(8):
        load_cast(K, wi[:, kc, :], I['w_in'][l, kc * 128:(kc + 1) * 128, :], f"wi{kc}", q="sp" if kc % 2 else "act")
    xt = [A.f32(D) for _ in range(2)]
    xn = [A.bf16(D) for _ in range(2)]
    st = [A.f32(4) for _ in range(2)]
    tmpf = [A.f32(D).rearrange("p (k t) -> p k t", k=8) for _ in range(2)]
    ust = [A.f32(DIN) for _ in range(2)]
    groups = [(i * 512, 512) for i in range(5)] + [(2560, 32)]
    gi = 0
    for tt in range(NT):
        s = tt % 2
        a = 1 if tt < 2 else 0
        rows = slice(tt * 128, (tt + 1) * 128)
        P.dma("sp", xt[s], K.xcur[rows, :], r=["xcur"], w=[f"xt{s}x"])
        norm_tile(K, xt[s], xn[s], st[s], f"xt{s}")
        tb = bank_bf(K, s).rearrange("p (k t) -> p k t", k=8)
        for kc in range(8):
            TR(P, tb[:, kc, :], xn[s][:, kc * 128:(kc + 1) * 128], K.ident_b[:, :],
               r=[f"xt{s}n", "ident_b"], w=[f"BK{s}"])
        TT(P, "dve", tmpf[s], tb, b3(K.G1[:, :, a], 128, 2), ALU.mult, r=[f"BK{s}", "G1"], w=[f"tmpf{s}"])
        TT(P, "pool", hT[:, :, rows], tmpf[s], b3(K.MOD[:, 0:8, a], 128, 2), ALU.add,
           r=[f"tmpf{s}", "MOD"], w=[f"hT{tt}"])
        for (c0, cw) in groups:
            bk = 2 + gi % 2
            for kc in range(8):
                MM(P, K.banks[bk][:, 0:cw], hT[:, kc, rows], wi[:, kc, c0:c0 + cw], kc == 0, kc == 7,
                   r=[f"hT{tt}", f"wi{kc}"], w=[f"BK{bk}"])
            CP(P, "act" if gi % 2 == 0 else "dve", ust[s][:, c0:c0 + cw], K.banks[bk][:, 0:cw],
               r=[f"BK{bk}"], w=[f"ust{s}"])
            gi += 1
        P.dma("sp", S['u'][rows, :], ust[s], r=[f"ust{s}"], w=["u"])
    P.barrier()
    A.release(m1)
    wg = A.bf16(8 * 3072).rearrange("p (k c) -> p k c", k=8)
    stage_init(K)
    for kc in range(8):
        load_cast(K, wg[:, kc, :], I['w_branch_gate'][l, kc * 128:(kc + 1) * 128, :], f"wg{kc}",
                  q="sp" if kc % 2 else "act")
    gst = [A.bf16(512) for _ in range(3)]
    gi = 0
    for (c0, n) in BLK:
        for j in range(24):
            bk = 4 + gi % 2
            for kc in range(8):
                MM(P, K.banks[bk][:, 0:n], wg[:, kc, j * 128:(j + 1) * 128], hT[:, kc, c0:c0 + n],
                   kc == 0, kc == 7, r=[f"wg{kc}"] + [f"hT{t}" for t in range(c0 // 128, (c0 + n) // 128)],
                   w=[f"BK{bk}"])
            g = gst[gi % 3]
            ACT(P, g[:, 0:n], K.banks[bk][:, 0:n], AF.Sigmoid, r=[f"BK{bk}", "bg"], w=[f"gst{gi % 3}"],
                bias=K.bg[:, j:j + 1])
            P.dma("sp", S['gatesT'][j, :, c0:c0 + n], g[:, 0:n], r=[f"gst{gi % 3}"], w=["gatesT"])
            gi += 1
    A.release(m0)


def rms_small(K, src, n, st, j, key, r):
    P = K.P
    ACT(P, K.junk[:, 0:n], src, AF.Square, r=r, w=["junk", key + "a"], accum_out=st[:, j:j + 1])
    ACT(P, st[:, 2 + j:3 + j], st[:, j:j + 1], AF.Sqrt, r=[key + "a"], w=[key + "b"], scale=1.0 / n, bias=EPS)
    RECIP(P, st[:, 4 + j:5 + j], st[:, 2 + j:3 + j], r=[key + "b"], w=[key + "c"])


def rope_apply(K, eng2, out, x, cc, ss, half, nh, r, w, tmpkey, tmp):
    P = K.P
    TT(P, "dve", tmp, x, b3(cc, nh, 1), ALU.mult, r=r, w=[tmpkey])
    TT(P, "dve", out[:, :, 0:half], x[:, :, half:2 * half], b3(ss[:, 0:half], nh, 1), ALU.mult, r=r, w=w)
    TT(P, "dve", out[:, :, half:2 * half], x[:, :, 0:half], b3(ss[:, half:2 * half], nh, 1), ALU.mult, r=r, w=w)
    TT(P, eng2, out, out, tmp, ALU.add, r=[tmpkey] + list(w), w=w)


def phase_mla_prep(K):
    P, A, I, S, l = K.P, K.A, K.I, K.S, K.l
    K.m_mla = A.mark()
    K.Vaug = A.bf16(NT * 8 * 65).rearrange("p (t h c) -> p t h c", t=NT, h=8)
    MEMSET(P, "pool", K.Vaug[:, :, :, 64:65], 1.0, r=[], w=["Vones"])
    m1 = A.mark()
    wuq = A.bf16(2 * 768).rearrange("p (k c) -> p k c", k=2)
    wukv = A.bf16(1024)
    stage_init(K)
    for c in range(2):
        load_cast(K, wuq[:, c, :], I['mla_w_uq'][l, c * 128:(c + 1) * 128, :], "wuq")
    load_cast(K, wukv, I['mla_w_ukv'][l, :, :], "wukv")
    qnw = A.f32(384)
    P.dma("sp", qnw[:, 0:256], I['mla_q_norm'][l:l + 1, :].broadcast_to([128, 256]), w=["qnw"])
    P.dma("sp", qnw[:, 256:384], I['mla_kv_norm'][l:l + 1, :].broadcast_to([128, 128]), w=["qnw"])
    K.junk = A.f32(1024)
    um = [A.f32(416) for _ in range(2)]
    ccm = [A.f32(32) for _ in range(2)]
    ssm = [A.f32(32) for _ in range(2)]
    st = [A.f32(8) for _ in range(2)]
    qkn = [A.bf16(384) for _ in range(2)]
    qkT = [A.bf16(384).rearrange("p (c t) -> p c t", c=3) for _ in range(2)]
    Qt = [A.bf16(768).rearrange("p (h d) -> p h d", h=8) for _ in range(2)]
    Kt = [A.bf16(768).rearrange("p (h d) -> p h d", h=8) for _ in range(2)]
    tmpq = [A.f32(128).rearrange("p (h d) -> p h d", h=4) for _ in range(2)]
    kr = [A.f32(32) for _ in range(2)]
    tmpk = [A.f32(32) for _ in range(2)]
    QTs = [A.bf16(1024).rearrange("p (h t) -> p h t", h=8) for _ in range(2)]
    KTs = [A.bf16(1024).rearrange("p (h t) -> p h t", h=8) for _ in range(2)]
    import os
    STOP = int(os.environ.get('MLA_STOP', '99'))
    NTT = int(os.environ.get('MLA_NT', str(NT)))
    for tt in range(NTT):
        s = tt % 2
        lat = tt >= 2
        rows = slice(tt * 128, (tt + 1) * 128)
        ks = f"m{s}"
        P.dma("sp", um[s], S['u'][rows, 1152:1568], r=["u"], w=[ks + "um"])
        if lat:
            tr = slice((tt - 2) * 128, (tt - 1) * 128)
            P.dma("sp", ccm[s], I['rope_m_c'][tr, :], w=[ks + "cs"])
            P.dma("sp", ssm[s], I['rope_m_s'][tr, :], w=[ks + "cs"])
        rms_small(K, um[s][:, 0:256], 256, st[s], 0, ks + "q", [ks + "um"])
        rms_small(K, um[s][:, 256:384], 128, st[s], 1, ks + "k", [ks + "um"])
        STT(P, qkn[s][:, 0:256], um[s][:, 0:256], st[s][:, 4:5], qnw[:, 0:256], ALU.mult, ALU.mult,
            r=[ks + "um", ks + "qc", "qnw"], w=[ks + "qkn"])
        STT(P, qkn[s][:, 256:384], um[s][:, 256:384], st[s][:, 5:6], qnw[:, 256:384], ALU.mult, ALU.mult,
            r=[ks + "um", ks + "kc", "qnw"], w=[ks + "qkn"])
        if STOP <= 1:
            continue
        tb = bank_bf(K, s)
        for c in range(3):
            TR(P, tb[:, c * 128:(c + 1) * 128], qkn[s][:, c * 128:(c + 1) * 128], K.ident_b[:, :],
               r=[ks + "qkn", "ident_b"], w=[f"BK{s}"])
        CP(P, "act", qkT[s], tb[:, 0:384].rearrange("p (c t) -> p c t", c=3), r=[f"BK{s}"], w=[ks + "qkT"])
        bq = [K.banks[2], K.banks[3]]
        bk = [K.banks[4], K.banks[5]]
        for cg in range(2):
            for c in range(2):
                MM(P, bq[cg][:, 0:384], qkT[s][:, c, :], wuq[:, c, cg * 384:(cg + 1) * 384], c == 0, c == 1,
                   r=[ks + "qkT", "wuq"], w=[f"BK{2 + cg}"])
        for cg in range(2):
            MM(P, bk[cg][:, 0:512], qkT[s][:, 2, :], wukv[:, cg * 512:(cg + 1) * 512], True, True,
               r=[ks + "qkT", "wukv"], w=[f"BK{4 + cg}"])
        if STOP <= 2:
            continue
        if lat:
            TT(P, "dve", tmpk[s], um[s][:, 384:416], ccm[s], ALU.mult, r=[ks + "um", ks + "cs"], w=[ks + "tmpk"])
            TT(P, "dve", kr[s][:, 0:16], um[s][:, 400:416], ssm[s][:, 0:16], ALU.mult, r=[ks + "um", ks + "cs"],
               w=[ks + "kr"])
            TT(P, "dve", kr[s][:, 16:32], um[s][:, 384:400], ssm[s][:, 16:32], ALU.mult,
               r=[ks + "um", ks + "cs"], w=[ks + "kr"])
            TT(P, "pool", kr[s], kr[s], tmpk[s], ALU.add, r=[ks + "tmpk", ks + "kr"], w=[ks + "kr"])
        else:
            CP(P, "pool", kr[s], um[s][:, 384:416], r=[ks + "um"], w=[ks + "kr"])
        for cg in range(2):
            hs = slice(4 * cg, 4 * cg + 4)
            q3 = bq[cg][:, 0:384].rearrange("p (h d) -> p h d", h=4)
            k3 = bk[cg][:, 0:512].rearrange("p (h d) -> p h d", h=4)
            if lat:
                CP(P, "act", Qt[s][:, hs, 0:64], q3[:, :, 0:64], r=[f"BK{2 + cg}"], w=[ks + "Qt"])
                rope_apply(K, "pool", Qt[s][:, hs, 64:96], q3[:, :, 64:96], ccm[s], ssm[s], 16, 4,
                           r=[f"BK{2 + cg}", ks + "cs"], w=[ks + "Qt"], tmpkey=ks + "tmpq", tmp=tmpq[s])
            else:
                CP(P, "act", Qt[s][:, hs, :], q3, r=[f"BK{2 + cg}"], w=[ks + "Qt"])
            CP(P, "act", Kt[s][:, hs, 0:64], k3[:, :, 0:64], r=[f"BK{4 + cg}"], w=[ks + "Kt"])
            CP(P, "dve", K.Vaug[:, tt, hs, 0:64], k3[:, :, 64:128], r=[f"BK{4 + cg}"], w=["V%d" % tt])
        CP(P, "pool", Kt[s][:, :, 64:96], b3(kr[s], 8, 1), r=[ks + "kr"], w=[ks + "Kt"])
        if STOP <= 3:
            continue
        tq = bank_bf(K, 6).rearrange("p (h t) -> p h t", h=8)
        tk = bank_bf(K, 7).rearrange("p (h t) -> p h t", h=8)
        for h in range(8):
            TR(P, tq[0:96, h, :], Qt[s][:, h, :], K.ident_b[:, :], r=[ks + "Qt", "ident_b"], w=["BK6"])
        for h in range(8):
            TR(P, tk[0:96, h, :], Kt[s][:, h, :], K.ident_b[:, :], r=[ks + "Kt", "ident_b"], w=["BK7"])
        CP(P, "act", QTs[s][0:96], tq[0:96], r=["BK6"], w=[ks + "QTs"])
        CP(P, "dve", KTs[s][0:96], tk[0:96], r=["BK7"], w=[ks + "KTs"])
        P.dma("sp", S['QT'][:, :, rows].rearrange("h d t -> d h t"), QTs[s][0:96], r=[ks + "QTs"], w=["QT"])
        P.dma("sp", S['KT'][:, :, rows].rearrange("h d t -> d h t"), KTs[s][0:96], r=[ks + "KTs"], w=["KT"])
    P.barrier()
    A.release(m1)


def phase_attn(K):
    P, A, I, S, l = K.P, K.A, K.I, K.S, K.l
    QTh = [A.bf16(T) for _ in range(2)]
    KTh = [A.bf16(T) for _ in range(2)]
    pt = [A.bf16(512) for _ in range(3)]
    osb = [A.f32(512) for _ in range(2)]
    rb = [A.f32(512) for _ in range(2)]
    ybs = [A.bf16(512) for _ in range(2)]
    scale = 96.0 ** -0.5
    qblocks = ([(0, 256, [0, 1])] if K.with_ctx else []) + [(256 + 512 * i, 512, list(range(NT))) for i in range(8)]
    cnt = 0
    qi = 0
    for h in range(8):
        s = h % 2
        P.dma("sp", QTh[s][0:96, :], S['QT'][h], r=["QT"], w=[f"QTh{s}"])
        P.dma("act", KTh[s][0:96, :], S['KT'][h], r=["KT"], w=[f"KTh{s}"])
        for (q0, qn, kts) in qblocks:
            ob = K.banks[4 + qi % 2]
            o2 = qi % 2

            def smm(kt, c):
                MM(P, K.banks[c % 2][:, 0:qn], KTh[s][0:96, kt * 128:(kt + 1) * 128], QTh[s][0:96, q0:q0 + qn],
                   True, True, r=[f"QTh{s}", f"KTh{s}"], w=[f"BK{c % 2}"])
            smm(kts[0], cnt)
            for i, kt in enumerate(kts):
                c = cnt + i
                if i + 1 < len(kts):
                    smm(kts[i + 1], c + 1)
                ACT(P, pt[c % 3][:, 0:qn], K.banks[c % 2][:, 0:qn], AF.Exp, r=[f"BK{c % 2}"], w=[f"pt{c % 3}"],
                    scale=scale)
                MM(P, ob[0:65, 0:qn], K.Vaug[:, kt, h, :], pt[c % 3][:, 0:qn], i == 0, i == len(kts) - 1,
                   r=[f"pt{c % 3}", "V%d" % kt, "Vones"], w=[f"BK{4 + o2}"])
            cnt += len(kts)
            CP(P, "dve", osb[o2][0:65, 0:qn], ob[0:65, 0:qn], r=[f"BK{4 + o2}"], w=[f"osb{o2}"])
            MM(P, K.banks[6][0:64, 0:qn], K.ones_f[64:65, 0:64], osb[o2][64:65, 0:qn], True, True,
               r=[f"osb{o2}", "ones_f"], w=["BK6"])
            RECIP(P, rb[o2][0:64, 0:qn], K.banks[6][0:64, 0:qn], r=["BK6"], w=[f"rb{o2}"])
            TT(P, "pool", ybs[o2][0:64, 0:qn], osb[o2][0:64, 0:qn], rb[o2][0:64, 0:qn], ALU.mult,
               r=[f"osb{o2}", f"rb{o2}"], w=[f"ybs{o2}"])
            P.dma("sp", S['ybT'][h, :, q0:q0 + qn], ybs[o2][0:64, 0:qn], r=[f"ybs{o2}"], w=["ybT"])
            qi += 1
    A.release(K.m_mla)


def phase_ret(K):
    P, A, I, S, l = K.P, K.A, K.I, K.S, K.l
    m0 = A.mark()
    C = 128
    KR = A.bf16(NT * 256).rearrange("p (t c) -> p t c", t=NT)
    VR = A.bf16(NT * 256).rearrange("p (t c) -> p t c", t=NT)
    RBst = A.bf16(NT * 256).rearrange("p (t h v) -> p t h v", t=NT, h=4)
    rel = A.f32(512).rearrange("p (a i) -> p a i", a=4)
    pos = A.f32(4)
    lg = A.f32(8)
    t8 = A.f32(8)
    gc8 = A.f32(8)
    Dc = A.f32(512).rearrange("p (h i) -> p h i", h=4)
    tA = A.f32(128)
    tB = A.f32(128)
    CFB = A.f32(16).rearrange("p (a h) -> p a h", a=4)
    GCf = A.f32(256).rearrange("p (h v) -> p h v", h=4)
    GCb = A.f32(256).rearrange("p (h v) -> p h v", h=4)
    P.dma("sp", rel, I['ret_rel'][:, :, :], w=["rel"])
    P.dma("sp", pos, I['ret_pos'][:, :], w=["pos"])
    P.dma("sp", lg, I['ret_decay'][l:l + 1].rearrange("a d h -> a (d h)").broadcast_to([128, 8]), w=["lg"])
    ACT(P, t8, lg, AF.Exp, r=["lg"], w=["t8"], scale=-1.0)
    ACT(P, t8, t8, AF.Ln, r=["t8"], w=["t8"], bias=1.0)
    TS(P, "dve", lg, t8, -1.0, None, ALU.mult, None, r=["t8"], w=["lg"])
    for h in range(4):
        ACT(P, tA, rel[:, 0, :], AF.Exp, r=["rel", "lg"], w=["tA"], scale=lg[:, h:h + 1])
        TT(P, "dve", tA, tA, rel[:, 1, :], ALU.mult, r=["tA", "rel"], w=["tA"])
        ACT(P, tB, rel[:, 2, :], AF.Exp, r=["rel", "lg"], w=["tB"], scale=lg[:, 4 + h:5 + h])
        TT(P, "dve", tB, tB, rel[:, 3, :], ALU.mult, r=["tB", "rel"], w=["tB"])
        TT(P, "dve", Dc[:, h, :], tA, tB, ALU.add, r=["tA", "tB"], w=["Dc"])
        for a in range(4):
            d = a % 2
            ACT(P, CFB[:, a, h:h + 1], pos[:, a:a + 1], AF.Exp, r=["pos", "lg"], w=["CFB"],
                scale=lg[:, 4 * d + h:4 * d + h + 1])
    ACT(P, gc8, lg, AF.Exp, r=["lg"], w=["gc8"], scale=float(C))
    CP(P, "dve", GCf, b3(gc8[:, 0:4], 64, 2), r=["gc8"], w=["GCf"])
    CP(P, "dve", GCb, b3(gc8[:, 4:8], 64, 2), r=["gc8"], w=["GCb"])
    Rf = A.f32(256).rearrange("p (h v) -> p h v", h=4)
    Rb = A.f32(256).rearrange("p (h v) -> p h v", h=4)
    Rfb = A.bf16(256).rearrange("p (h v) -> p h v", h=4)
    tmpR = A.f32(256).rearrange("p (h v) -> p h v", h=4)
    MEMSET(P, "pool", Rf, 0.0, r=[], w=["Rf"])
    MEMSET(P, "pool", Rb, 0.0, r=[], w=["Rb"])
    MEMSET(P, "pool", Rfb, 0.0, r=[], w=["Rfb"])
    ukv = [A.f32(512) for _ in range(2)]
    uqg = [A.f32(512) for _ in range(2)]
    ccr = [A.f32(64) for _ in range(2)]
    ssr = [A.f32(64) for _ in range(2)]
    tmpr = [A.f32(256).rearrange("p (h d) -> p h d", h=4) for _ in range(2)]
    Ktl = [A.bf16(256).rearrange("p (h d) -> p h d", h=4) for _ in range(2)]
    qr = [A.bf16(256).rearrange("p (h d) -> p h d", h=4) for _ in range(2)]
    QKT = [A.bf16(1024).rearrange("p (h t) -> p h t", h=8) for _ in range(2)]
    SM = [A.bf16(512).rearrange("p (h t) -> p h t", h=4) for _ in range(2)]
    O1 = [A.f32(256).rearrange("p (h d) -> p h d", h=4) for _ in range(2)]
    O2 = [A.f32(256).rearrange("p (h d) -> p h d", h=4) for _ in range(2)]
    sq = [A.f32(256).rearrange("p (h d) -> p h d", h=4) for _ in range(2)]
    sst = [A.f32(16) for _ in range(2)]
    sg = [A.f32(256) for _ in range(2)]
    yc = [A.bf16(256) for _ in range(2)]
    ycTs = [A.bf16(256).rearrange("p (c t) -> p c t", c=2) for _ in range(2)]
    b0, b1, b2, b3_, b4 = K.banks[0], K.banks[1], K.banks[2], K.banks[3], K.banks[4]

    def tables(tt, s, kname, key):
        tr = slice((tt - 2) * 128, (tt - 1) * 128)
        P.dma("sp", ccr[s], I[kname + '_c'][tr, :], w=[key])
        P.dma("sp", ssr[s], I[kname + '_s'][tr, :], w=[key])

    def state_update(R, Rbf, GC, tt, tailidx, s, key):
        TT(P, "dve", Ktl[s], KR[:, tt, :].rearrange("p (h d) -> p h d", h=4), b3(CFB[:, tailidx, :], 64, 2),
           ALU.mult, r=[f"KR{tt}", "CFB"], w=[f"Ktl{s}"])
        for h in range(4):
            MM(P, b3_[0:64, 256 + h * 64:256 + (h + 1) * 64], Ktl[s][:, h, :], VR[:, tt, h * 64:(h + 1) * 64],
               True, True, r=[f"Ktl{s}", f"VR{tt}"], w=["BK3"])
        TT(P, "dve", tmpR[0:64], R[0:64], GC[0:64], ALU.mult, r=[key, "GCf", "GCb"], w=["tmpR"])
        TT(P, "dve", R[0:64], tmpR[0:64], b3_[0:64, 256:512].rearrange("p (h v) -> p h v", h=4), ALU.add,
           r=["tmpR", "BK3"], w=[key])
        if Rbf is not None:
            CP(P, "pool", Rbf[0:64], R[0:64], r=[key], w=[key + "b"])

    order_b = [1, 0] + list(range(NT - 1, 1, -1))
    for n, tt in enumerate(order_b):
        s = n % 2
        rows = slice(tt * 128, (tt + 1) * 128)
        P.dma("sp", ukv[s], S['u'][rows, 1824:2336], r=["u"], w=[f"ukv{s}"])
        k3 = ukv[s][:, 0:256].rearrange("p (h d) -> p h d", h=4)
        if tt >= 2:
            tables(tt, s, 'rope_rk', f"cs{s}")
            rope_apply(K, "pool", KR[:, tt, :].rearrange("p (h d) -> p h d", h=4), k3, ccr[s], ssr[s], 32, 4,
                       r=[f"ukv{s}", f"cs{s}"], w=[f"KR{tt}"], tmpkey=f"tmpr{s}", tmp=tmpr[s])
        else:
            ACT(P, KR[:, tt, :], ukv[s][:, 0:256], AF.Identity, r=[f"ukv{s}"], w=[f"KR{tt}"], scale=0.125)
        CP(P, "pool", VR[:, tt, :], ukv[s][:, 256:512], r=[f"ukv{s}"], w=[f"VR{tt}"])
        CP(P, "act", RBst[0:64, tt], Rb[0:64], r=["Rb"], w=[f"RBst{tt}"])
        if n < NT - 1:
            state_update(Rb, None, GCb, tt, 3, s, "Rb")
    for tt in range(NT):
        s = tt % 2
        rows = slice(tt * 128, (tt + 1) * 128)
        P.dma("sp", uqg[s][:, 0:256], S['u'][rows, 1568:1824], r=["u"], w=[f"uqg{s}"])
        P.dma("sp", uqg[s][:, 256:512], S['u'][rows, 2336:2592], r=["u"], w=[f"uqg{s}"])
        q3 = uqg[s][:, 0:256].rearrange("p (h d) -> p h d", h=4)
        if tt >= 2:
            tables(tt, s, 'rope_r', f"cs{s}")
            rope_apply(K, "pool", qr[s], q3, ccr[s], ssr[s], 32, 4, r=[f"uqg{s}", f"cs{s}"], w=[f"qr{s}"],
                       tmpkey=f"tmpr{s}", tmp=tmpr[s])
        else:
            CP(P, "pool", qr[s], q3, r=[f"uqg{s}"], w=[f"qr{s}"])
        tqk = bank_bf(K, 0).rearrange("p (h t) -> p h t", h=8)
        for h in range(4):
            TR(P, tqk[0:64, h, :], qr[s][:, h, :], K.ident_b[:, :], r=[f"qr{s}", "ident_b"], w=["BK0"])
        for h in range(4):
            TR(P, tqk[0:64, 4 + h, :], KR[:, tt, h * 64:(h + 1) * 64], K.ident_b[:, :], r=[f"KR{tt}", "ident_b"],
               w=["BK0"])
        CP(P, "act", QKT[s][0:64], tqk[0:64], r=["BK0"], w=[f"QKT{s}"])
        for h in range(4):
            MM(P, b1[:, h * 128:(h + 1) * 128], QKT[s][0:64, 4 + h, :], QKT[s][0:64, h, :], True, True,
               r=[f"QKT{s}"], w=["BK1"])
        TT(P, "dve", SM[s], b1[:, :].rearrange("p (h t) -> p h t", h=4), Dc, ALU.mult, r=["BK1", "Dc"],
           w=[f"SM{s}"])
        for h in range(4):
            MM(P, b2[:, h * 64:(h + 1) * 64], SM[s][:, h, :], VR[:, tt, h * 64:(h + 1) * 64], True, True,
               r=[f"SM{s}", f"VR{tt}"], w=["BK2"])
        for h in range(4):
            MM(P, b2[:, 256 + h * 64:256 + (h + 1) * 64], QKT[s][0:64, h, :], Rfb[0:64, h, :], True, True,
               r=[f"QKT{s}", "Rfb"], w=["BK2"])
        for h in range(4):
            MM(P, b3_[:, h * 64:(h + 1) * 64], QKT[s][0:64, h, :], RBst[0:64, tt, h, :], True, True,
               r=[f"QKT{s}", f"RBst{tt}"], w=["BK3"])
        TT(P, "dve", O1[s], b2[:, 256:512].rearrange("p (h d) -> p h d", h=4), b3(CFB[:, 0, :], 64, 2), ALU.mult,
           r=["BK2", "CFB"], w=[f"O1{s}"])
        TT(P, "dve", O2[s], b3_[:, 0:256].rearrange("p (h d) -> p h d", h=4), b3(CFB[:, 1, :], 64, 2), ALU.mult,
           r=["BK3", "CFB"], w=[f"O2{s}"])
        TT(P, "dve", O1[s], O1[s], b2[:, 0:256].rearrange("p (h d) -> p h d", h=4), ALU.add,
           r=["BK2", f"O1{s}"], w=[f"O1{s}"])
        TT(P, "pool", O1[s], O1[s], O2[s], ALU.add, r=[f"O1{s}", f"O2{s}"], w=[f"O1{s}"])
        state_update(Rf, Rfb, GCf, tt, 2, s, "Rf")
        TT(P, "pool", sq[s], O1[s], O1[s], ALU.mult, r=[f"O1{s}"], w=[f"sq{s}"])
        RED(P, sst[s][:, 0:4], sq[s], ALU.add, r=[f"sq{s}"], w=[f"sst{s}"])
        ACT(P, sst[s][:, 4:8], sst[s][:, 0:4], AF.Sqrt, r=[f"sst{s}"], w=[f"sst{s}b"], scale=1.0 / 64, bias=EPS)
        RECIP(P, sst[s][:, 8:12], sst[s][:, 4:8], r=[f"sst{s}b"], w=[f"sst{s}c"])
        ACT(P, sg[s], uqg[s][:, 256:512], AF.Silu, r=[f"uqg{s}"], w=[f"sg{s}"])
        TT(P, "dve", O1[s], O1[s], b3(sst[s][:, 8:12], 64, 2), ALU.mult, r=[f"O1{s}", f"sst{s}c"], w=[f"O1{s}"])
        TT(P, "pool", yc[s], O1[s].rearrange("p h d -> p (h d)"), sg[s], ALU.mult, r=[f"O1{s}", f"sg{s}"],
           w=[f"yc{s}"])
        ty = bank_bf(K, 4)
        for c in range(2):
            TR(P, ty[:, c * 128:(c + 1) * 128], yc[s][:, c * 128:(c + 1) * 128], K.ident_b[:, :],
               r=[f"yc{s}", "ident_b"], w=["BK4"])
        CP(P, "act", ycTs[s], ty[:, 0:256].rearrange("p (c t) -> p c t", c=2), r=["BK4"], w=[f"ycTs{s}"])
        P.dma("sp", S['ycT'][:, :, rows].rearrange("c p t -> p c t"), ycTs[s], r=[f"ycTs{s}"], w=["ycT"])
    A.release(m0)


def phase_rwkv_prep(K):
    P, A, I, S, l = K.P, K.A, K.I, K.S, K.l
    m0 = A.mark()
    CW = A.f32(2304).rearrange("p (j c) -> p j c", j=3)
    P.dma("sp", CW, I['rwkv_conv'][l:l + 1].rearrange("a j c -> a (j c)").broadcast_to([128, 2304]), w=["CW"])
    ROW = A.f32(256 * 7)
    KKb, KAb, RKb = ROW[:, 0:256], ROW[:, 256:512], ROW[:, 512:768]
    W0b = ROW[:, 768:1280].rearrange("p (d c) -> p d c", d=2)
    A0b = ROW[:, 1280:1792].rearrange("p (d c) -> p d c", d=2)
    P.dma("sp", KKb, I['rwkv_k_k'][l:l + 1, :].broadcast_to([128, 256]), w=["ROW"])
    P.dma("sp", KAb, I['rwkv_k_a'][l:l + 1, :].broadcast_to([128, 256]), w=["ROW"])
    P.dma("sp", RKb, I['rwkv_r_k'][l:l + 1].rearrange("a h d -> a (h d)").broadcast_to([128, 256]), w=["ROW"])
    P.dma("sp", ROW[:, 768:1280], I['rwkv_w0'][l:l + 1].rearrange("a d c -> a (d c)").broadcast_to([128, 512]),
          w=["ROW"])
    P.dma("sp", ROW[:, 1280:1792], I['rwkv_a0'][l:l + 1].rearrange("a d c -> a (d c)").broadcast_to([128, 512]),
          w=["ROW"])
    gup = A.bf16(256)
    wup = A.bf16(256)
    aup = A.bf16(256)
    stage_init(K)
    load_cast(K, gup, I['rwkv_g_up'][l, :, :], "gup")
    load_cast(K, wup, I['rwkv_w_up'][l].rearrange("d k c -> (d k) c"), "wup")
    load_cast(K, aup, I['rwkv_a_up'][l].rearrange("d k c -> (d k) c"), "aup")
    U0 = [A.f32(1152) for _ in range(2)]
    UM = [A.f32(768) for _ in range(2)]
    UP = [A.f32(768) for _ in range(2)]
    rkv = [A.f32(768) for _ in range(2)]
    t1 = [A.f32(768) for _ in range(2)]
    X3 = [A.bf16(384) for _ in range(2)]
    X3T = [A.bf16(384).rearrange("p (c t) -> p c t", c=3) for _ in range(2)]
    kkr = [A.f32(256) for _ in range(2)]
    sqk = [A.f32(256) for _ in range(2)]
    nkk = [A.f32(256) for _ in range(2)]
    stt_ = [A.f32(16) for _ in range(2)]
    zt = [A.f32(256) for _ in range(2)]
    al = [A.f32(256) for _ in range(2)]
    ST = [[A.f32(2048).rearrange("p (h q c) -> p h q c", h=4, q=8) for _ in range(2)] for _ in range(2)]
    GB = [A.f32(260) for _ in range(2)]
    bA, bB, bC = K.banks[2], K.banks[3], K.banks[4]

    def v4(ap):
        return ap.rearrange("p (h c) -> p h c", h=4)
    for tt in range(NT):
        s = tt % 2
        ks = f"w{s}"
        t0 = tt * 128
        rows = slice(t0, t0 + 128)
        seg_start = tt in (0, 2)
        seg_end = tt in (1, NT - 1)
        P.dma("sp", U0[s], S['u'][rows, 0:1152], r=["u"], w=[ks + "U0"])
        if seg_start:
            MEMSET(P, "pool", UM[s][0:32, :], 0.0, r=[], w=[ks + "UM"])
            P.dma("sp", UM[s][1:128, :], S['u'][t0:t0 + 127, 0:768], r=["u"], w=[ks + "UM"])
        else:
            P.dma("sp", UM[s], S['u'][t0 - 1:t0 + 127, 0:768], r=["u"], w=[ks + "UM"])
        if seg_end:
            MEMSET(P, "pool", UP[s][96:128, :], 0.0, r=[], w=[ks + "UP"])
            P.dma("sp", UP[s][0:127, :], S['u'][t0 + 1:t0 + 128, 0:768], r=["u"], w=[ks + "UP"])
        else:
            P.dma("sp", UP[s], S['u'][t0 + 1:t0 + 129, 0:768], r=["u"], w=[ks + "UP"])
        TT(P, "pool", rkv[s], UM[s], CW[:, 0, :], ALU.mult, r=[ks + "UM", "CW"], w=[ks + "rkv"])
        TT(P, "pool", t1[s], U0[s][:, 0:768], CW[:, 1, :], ALU.mult, r=[ks + "U0", "CW"], w=[ks + "t1"])
        TT(P, "dve", rkv[s], rkv[s], t1[s], ALU.add, r=[ks + "rkv", ks + "t1"], w=[ks + "rkv"])
        TT(P, "pool", t1[s], UP[s], CW[:, 2, :], ALU.mult, r=[ks + "UP", "CW"], w=[ks + "t1"])
        TT(P, "dve", rkv[s], rkv[s], t1[s], ALU.add, r=[ks + "rkv", ks + "t1"], w=[ks + "rkv"])
        r_, k_, v_ = rkv[s][:, 0:256], rkv[s][:, 256:512], rkv[s][:, 512:768]
        ACT(P, X3[s][:, 0:128], U0[s][:, 1024:1152], AF.Sigmoid, r=[ks + "U0"], w=[ks + "X3"])
        ACT(P, X3[s][:, 128:256], U0[s][:, 768:896], AF.Tanh, r=[ks + "U0"], w=[ks + "X3"])
        CP(P, "dve", X3[s][:, 256:384], U0[s][:, 896:1024], r=[ks + "U0"], w=[ks + "X3"])
        tb = bank_bf(K, s)
        for c in range(3):
            TR(P, tb[:, c * 128:(c + 1) * 128], X3[s][:, c * 128:(c + 1) * 128], K.ident_b[:, :],
               r=[ks + "X3", "ident_b"], w=[f"BK{s}"])
        CP(P, "act", X3T[s], tb[:, 0:384].rearrange("p (c t) -> p c t", c=3), r=[f"BK{s}"], w=[ks + "X3T"])
        MM(P, bA[:, 0:256], X3T[s][:, 0, :], gup, True, True, r=[ks + "X3T", "gup"], w=["BK2"])
        MM(P, bA[:, 256:512], X3T[s][0:64, 1, :], wup[0:64, :], True, True, r=[ks + "X3T", "wup"], w=["BK2"])
        MM(P, bB[:, 0:256], X3T[s][64:128, 1, :], wup[64:128, :], True, True, r=[ks + "X3T", "wup"], w=["BK3"])
        MM(P, bB[:, 256:512], X3T[s][0:64, 2, :], aup[0:64, :], True, True, r=[ks + "X3T", "aup"], w=["BK3"])
        MM(P, bC[:, 0:256], X3T[s][64:128, 2, :], aup[64:128, :], True, True, r=[ks + "X3T", "aup"], w=["BK4"])
        wl = [bA[:, 256:512], bB[:, 0:256]]
        alo = [bB[:, 256:512], bC[:, 0:256]]
        wlk = ["BK2", "BK3"]
        alk = ["BK3", "BK4"]
        TT(P, "pool", kkr[s], k_, KKb, ALU.mult, r=[ks + "rkv", "ROW"], w=[ks + "kkr"])
        TT(P, "pool", sqk[s], kkr[s], kkr[s], ALU.mult, r=[ks + "kkr"], w=[ks + "sqk"])
        RED(P, stt_[s][:, 0:4], v4(sqk[s]), ALU.add, r=[ks + "sqk"], w=[ks + "st0"])
        ACT(P, stt_[s][:, 4:8], stt_[s][:, 0:4], AF.Sqrt, r=[ks + "st0"], w=[ks + "st1"])
        TS(P, "dve", stt_[s][:, 4:8], stt_[s][:, 4:8], 1e-12, None, ALU.max, None, r=[ks + "st1"], w=[ks + "st1"])
        RECIP(P, stt_[s][:, 8:12], stt_[s][:, 4:8], r=[ks + "st1"], w=[ks + "st2"])
        STT(P, v4(nkk[s]), v4(kkr[s]), -1.0, b3(stt_[s][:, 8:12], 64, 2), ALU.mult, ALU.mult,
            r=[ks + "kkr", ks + "st2"], w=[ks + "nkk"])
        CP(P, "act", GB[s][:, 0:256], bA[:, 0:256], r=["BK2"], w=[ks + "GB"])
        TT(P, "pool", sqk[s], r_, k_, ALU.mult, r=[ks + "rkv", ks + "st0"], w=[ks + "sqk"])
        TT(P, "pool", sqk[s], sqk[s], RKb, ALU.mult, r=[ks + "sqk", "ROW"], w=[ks + "sqk"])
        RED(P, GB[s][:, 256:260], v4(sqk[s]), ALU.add, r=[ks + "sqk"], w=[ks + "GB"])
        for d in range(2):
            st = ST[d][s]
            kd = ks + f"ST{d}"
            TT(P, "dve", zt[s], wl[d], W0b[:, d, :], ALU.add, r=[wlk[d], "ROW"], w=[ks + "zt"])
            ACT(P, zt[s], zt[s], AF.Exp, r=[ks + "zt"], w=[ks + "zt"], scale=-1.0)
            ACT(P, zt[s], zt[s], AF.Ln, r=[ks + "zt"], w=[ks + "zt"], bias=1.0)
            ACT(P, st[:, :, 0, :], v4(zt[s]), AF.Exp, r=[ks + "zt"], w=[kd], scale=-1.0, bias=-0.5)
            TT(P, "dve", al[s], alo[d], A0b[:, d, :], ALU.add, r=[alk[d], "ROW"], w=[ks + "al"])
            ACT(P, al[s], al[s], AF.Sigmoid, r=[ks + "al"], w=[ks + "al"])
            STT(P, st[:, :, 2, :], v4(nkk[s]), -1.0, v4(al[s]), ALU.mult, ALU.mult, r=[ks + "nkk", ks + "al"], w=[kd])
            STT(P, zt[s], al[s], -1.0, KAb, ALU.add, ALU.mult, r=[ks + "al", "ROW", ks + "zt"], w=[ks + "zt"])
            STT(P, st[:, :, 3, :], v4(zt[s]), 1.0, v4(k_), ALU.add, ALU.mult, r=[ks + "zt", ks + "rkv"], w=[kd])
            CP(P, "pool", st[:, :, 5:7, :], st[:, :, 2:4, :], r=[kd], w=[kd])
            CP(P, "pool", st[:, :, 1, :], v4(nkk[s]), r=[ks + "nkk"], w=[kd])
            CP(P, "pool", st[:, :, 4, :], v4(r_), r=[ks + "rkv"], w=[kd])
            CP(P, "act", st[:, :, 7, :], v4(v_), r=[ks + "rkv"], w=[kd])
            P.dma("sp", S[f'rwp{d}'][:, rows, :].rearrange("h t c -> t h c"),
                  st.rearrange("p h q c -> p h (q c)"), r=[kd], w=[f"rwp{d}"])
        P.dma("sp", S['gb'][rows, :], GB[s], r=[ks + "GB"], w=["gb"])
    A.release(m0)


def phase_rwkv_scan(K):
    P, A, I, S, l = K.P, K.A, K.I, K.S, K.l
    m0 = A.mark()
    NG = 4
    CM = A.f32(512).rearrange("p (a s) -> p a s", a=4)
    MKA = A.f32(512).rearrange("p (a s) -> p a s", a=4)
    MKC = A.f32(128)
    HMt = A.f32(8)
    IDB = A.f32(256).rearrange("p (h k) -> p h k", h=4)
    P.dma("sp", HMt, I['rw_hm'][:, :], w=["HM"])
    P.dma("sp", IDB[0:64], I['identb'][:, :, :], w=["IDB"])
    HM = HMt[:, 0:4]
    HMn = HMt[:, 4:8]
    Zin = [A.f32(NG * 512).rearrange("p (n c) -> p n c", n=NG) for _ in range(2)]
    Yst = [A.f32(NG * 64).rearrange("p (n c) -> p n c", n=NG) for _ in range(2)]
    Sst = [A.f32(256).rearrange("p (h v) -> p h v", h=4) for _ in range(2)]

    def slot_bufs():
        B = Ctx()
        B.E = A.f32(384)
        B.Z = A.f32(384).rearrange("p (q c) -> p q c", q=6)
        B.ZT = A.f32(512).rearrange("p (q c) -> p q c", q=4)
        B.MS = A.f32(512).rearrange("p (q c) -> p q c", q=4)
        B.Mm = A.f32(128)
        B.SQ = [A.f32(256).rearrange("p (q c) -> p q c", q=2) for _ in range(3)]
        B.SQ4 = A.f32(128)
        B.X = [A.f32(128) for _ in range(2)]
        B.Bblk = A.f32(256)
        B.Ublk = A.f32(256)
        B.Vblk = A.f32(256)
        B.Dg = A.f32(256)
        B.GT = A.f32(256).rearrange("p (h k) -> p h k", h=4)
        B.Hs = A.f32(256)
        B.QT = A.f32(128)
        B.Y0 = A.f32(64)
        B.gT = A.f32(4)
        return B
    SL = [slot_bufs() for _ in range(NG)]
    bk = K.banks
    gcount = 0
    for d in range(2):
        P.dma("sp", CM, I['rw_cm'][d], w=["CM"])
        P.dma("sp", MKA, I['rw_mka'][d], w=["MKA"])
        P.dma("sp", MKC, I['rw_mkc'][d], w=["MKC"])
        MEMSET(P, "pool", Sst[0], 0.0, r=[], w=["S0"])
        scur = 0
        if d == 0:
            groups = [g * 128 for g in range(T // 128)]
        else:
            groups = [128, 0] + [256 + g * 128 for g in range(31, -1, -1)]
        for tokmin in groups:
            zs = gcount % 2
            gcount += 1
            zk = f"Zin{zs}"
            for h in range(4):
                P.dma("sp" if h % 2 == 0 else "act", Zin[zs][h * 32:(h + 1) * 32, :, :],
                      S[f'rwp{d}'][h, tokmin:tokmin + 128, :].rearrange("(n s) c -> s n c", s=32),
                      r=[f"rwp{d}"], w=[zk])
            nat = [i if d == 0 else NG - 1 - i for i in range(NG)]
            for i in range(NG):
                B, Zi = SL[i], Zin[zs][:, nat[i], :]
                for q, cm in enumerate([0, 1, 1, 2, 3, 3]):
                    MM(P, bk[0][:, q * 64:(q + 1) * 64], CM[:, cm, :], Zi[:, 0:64], True, True,
                       r=["CM", zk], w=["BK0"])
                MM(P, bk[0][0:64, 384:388], Zi[:, 0:64], HMn, True, True, r=[zk, "HM"], w=["BK0"])
                ACT(P, B.E, bk[0][:, 0:384], AF.Exp, r=["BK0"], w=[f"E{i}"])
                ACT(P, B.gT[0:64], bk[0][0:64, 384:388], AF.Exp, r=["BK0"], w=[f"gT{i}"])
                TT(P, "pool", B.Z.rearrange("p q c -> p (q c)"), Zi[:, 64:448], B.E, ALU.mult,
                   r=[zk, f"E{i}"], w=[f"Z{i}"])
            for i in range(NG):
                B = SL[i]
                for q, zsl in enumerate([0, 3, 1, 2]):
                    TR(P, bk[1][0:64, q * 128:(q + 1) * 128], B.Z[:, zsl, :], K.ident_f[:, :],
                       r=[f"Z{i}", "ident_f"], w=["BK1"])
                CP(P, "act", B.ZT[0:64].rearrange("p q c -> p (q c)"), bk[1][0:64, :], r=["BK1"], w=[f"ZT{i}"])
            for i in range(NG):
                B = SL[i]
                AR = B.ZT[0:64, 0:2, :].rearrange("p q c -> p (q c)")
                MM(P, bk[2][:, 0:256], B.ZT[0:64, 2, :], AR, True, True, r=[f"ZT{i}"], w=["BK2"])
                MM(P, bk[2][:, 256:512], B.ZT[0:64, 3, :], AR, True, True, r=[f"ZT{i}"], w=["BK2"])
                MM(P, bk[3][:, 0:128], B.ZT[0:64, 0, :], B.ZT[0:64, 2, :], True, True, r=[f"ZT{i}"], w=["BK3"])
                TT(P, "dve", B.MS.rearrange("p q c -> p (q c)"), bk[2][:, :], MKA.rearrange("p a s -> p (a s)"),
                   ALU.mult, r=["BK2", "MKA"], w=[f"MS{i}"])
                TT(P, "dve", B.Mm, bk[3][:, 0:128], MKC, ALU.mult, r=["BK3", "MKC"], w=[f"Mm{i}"])
            for lev in range(4):
                for i in range(NG):
                    B = SL[i]
                    if lev == 0:
                        Pn, Pt = B.Mm, B.MS[:, 0, :]
                        rk = [f"Mm{i}", f"MS{i}"]
                    else:
                        Pn, Pt = B.SQ[lev - 1][:, 1, :], B.SQ[lev - 1][:, 0, :]
                        rk = [f"SQ{lev - 1}_{i}"]
                    MM(P, bk[4][:, 0:128], Pn, Pt, True, True, r=rk, w=["BK4"])
                    if lev < 3:
                        MM(P, bk[4][:, 128:256], Pt, Pn, True, True, r=rk, w=["BK4"])
                        CP(P, "act", B.SQ[lev].rearrange("p q c -> p (q c)"), bk[4][:, 0:256], r=["BK4"],
                           w=[f"SQ{lev}_{i}"])
                    else:
                        CP(P, "act", B.SQ4, bk[4][:, 0:128], r=["BK4"], w=[f"SQ3_{i}"])
            for i in range(NG):
                B, Zi = SL[i], Zin[zs][:, nat[i], :]
                MM(P, bk[3][:, 128:192], B.MS[:, 2, :], Zi[:, 448:512], True, True, r=[f"MS{i}", zk], w=["BK3"])
                CP(P, "dve", B.X[0][:, 64:128], bk[3][:, 128:192], r=["BK3"], w=[f"X0_{i}"])
                CP(P, "pool", B.X[0][:, 0:64], B.Z[:, 0, :], r=[f"Z{i}"], w=[f"X0_{i}"])
            for lev in range(5):
                for i in range(NG):
                    B = SL[i]
                    if lev == 0:
                        Pt, rk = B.MS[:, 0, :], f"MS{i}"
                    elif lev < 4:
                        Pt, rk = B.SQ[lev - 1][:, 0, :], f"SQ{lev - 1}_{i}"
                    else:
                        Pt, rk = B.SQ4, f"SQ3_{i}"
                    xi, xo = B.X[lev % 2], B.X[(lev + 1) % 2]
                    MM(P, bk[5][:, 0:128], Pt, xi, True, True, r=[rk, f"X{lev % 2}_{i}"], w=["BK5"])
                    TT(P, "dve", xo, bk[5][:, 0:128], xi, ALU.add, r=["BK5", f"X{lev % 2}_{i}"],
                       w=[f"X{(lev + 1) % 2}_{i}"])
            for i in range(NG):
                B, Zi = SL[i], Zin[zs][:, nat[i], :]
                WU, wk = B.X[1], f"X1_{i}"
                HM3 = b3(HM, 64, 2)
                TT(P, "pool", B.Bblk.rearrange("p (h c) -> p h c", h=4), b3(B.Z[:, 4, :], 4, 1), HM3, ALU.mult,
                   r=[f"Z{i}", "HM"], w=[f"Bblk{i}"])
                TT(P, "pool", B.Ublk.rearrange("p (h c) -> p h c", h=4), b3(WU[:, 64:128], 4, 1), HM3, ALU.mult,
                   r=[wk, "HM"], w=[f"Ublk{i}"])
                TT(P, "pool", B.Vblk.rearrange("p (h c) -> p h c", h=4), b3(Zi[:, 448:512], 4, 1), HM3, ALU.mult,
                   r=[zk, "HM"], w=[f"Vblk{i}"])
                TT(P, "pool", B.Dg[0:64].rearrange("p (h c) -> p h c", h=4), IDB[0:64], b3(B.gT[0:64], 64, 2),
                   ALU.mult, r=["IDB", f"gT{i}"], w=[f"Dg{i}"])
                MM(P, bk[6][0:64, 0:256], WU[:, 0:64], B.Bblk, True, True, r=[wk, f"Bblk{i}"], w=["BK6"])
                TT(P, "dve", B.GT[0:64].rearrange("p h k -> p (h k)"), bk[6][0:64, 0:256], B.Dg[0:64], ALU.add,
                   r=["BK6", f"Dg{i}"], w=[f"GT{i}"])
                MM(P, bk[6][0:64, 256:512], B.Z[:, 4, :], B.Ublk, True, False, r=[f"Z{i}", f"Ublk{i}"], w=["BK6"])
                MM(P, bk[6][0:64, 256:512], B.Z[:, 5, :], B.Vblk, False, True, r=[f"Z{i}", f"Vblk{i}"], w=["BK6"])
                CP(P, "act", B.Hs[0:64], bk[6][0:64, 256:512], r=["BK6"], w=[f"Hs{i}"])
                MM(P, bk[7][0:64, 0:128], WU[:, 0:64], B.MS[:, 1, :], True, True, r=[wk, f"MS{i}"], w=["BK7"])
                TT(P, "dve", B.QT[0:64], bk[7][0:64, 0:128], B.ZT[0:64, 1, :], ALU.add, r=["BK7", f"ZT{i}"],
                   w=[f"QT{i}"])
                MM(P, bk[7][:, 128:192], B.MS[:, 1, :], WU[:, 64:128], True, False, r=[wk, f"MS{i}"], w=["BK7"])
                MM(P, bk[7][:, 128:192], B.MS[:, 3, :], Zi[:, 448:512], False, True, r=[zk, f"MS{i}"], w=["BK7"])
                CP(P, "act", B.Y0, bk[7][:, 128:192], r=["BK7"], w=[f"Y0{i}"])
            ys = zs
            for i in range(NG):
                B = SL[i]
                Sc, Sn = Sst[scur], Sst[1 - scur]
                for h in range(4):
                    MM(P, bk[5][0:64, 128 + h * 64:128 + (h + 1) * 64], B.GT[0:64, h, :], Sc[0:64, h, :], True, True,
                       r=[f"GT{i}", f"S{scur}"], w=["BK5"])
                for h in (0, 1):
                    MM(P, bk[5][h * 32:(h + 1) * 32, 384:448], B.QT[0:64, h * 32:(h + 1) * 32], Sc[0:64, h, :],
                       True, True, r=[f"QT{i}", f"S{scur}"], w=["BK5"])
                MM(P, bk[5][64:128, 384:448], B.QT[0:64, 64:128], Sc[0:64, 3, :], True, True,
                   r=[f"QT{i}", f"S{scur}"], w=["BK5"])
                MM(P, bk[5][64:96, 384:448], B.QT[0:64, 64:96], Sc[0:64, 2, :], True, True,
                   r=[f"QT{i}", f"S{scur}"], w=["BK5"])
                TT(P, "dve", Sn[0:64].rearrange("p h v -> p (h v)"), bk[5][0:64, 128:384], B.Hs[0:64], ALU.add,
                   r=["BK5", f"Hs{i}"], w=[f"S{1 - scur}"])
                TT(P, "dve", Yst[ys][:, nat[i], :], bk[5][:, 384:448], B.Y0, ALU.add, r=["BK5", f"Y0{i}"],
                   w=[f"Yst{ys}"])
                scur = 1 - scur
            for h in range(4):
                P.dma("sp" if h % 2 == 0 else "act",
                      S[f'yd{d}'][tokmin:tokmin + 128, h * 64:(h + 1) * 64].rearrange("(n s) v -> s n v", s=32),
                      Yst[ys][h * 32:(h + 1) * 32, :, :], r=[f"Yst{ys}"], w=[f"yd{d}"])
        P.barrier()
    A.release(m0)


def phase_rwkv_out(K):
    P, A, I, S, l = K.P, K.A, K.I, K.S, K.l
    m0 = A.mark()
    ROW = A.f32(512)
    P.dma("sp", ROW[:, 0:256], I['rwkv_ln_w'][l:l + 1, :].broadcast_to([128, 256]), w=["ROW"])
    P.dma("sp", ROW[:, 256:512], I['rwkv_ln_b'][l:l + 1, :].broadcast_to([128, 256]), w=["ROW"])
    yf = [A.f32(256) for _ in range(2)]
    yb = [A.f32(256) for _ in range(2)]
    gbt = [A.f32(260) for _ in range(2)]
    vt = [A.f32(256) for _ in range(2)]
    sq = [A.f32(256) for _ in range(2)]
    st = [A.f32(24) for _ in range(2)]
    ya = [A.bf16(256) for _ in range(2)]
    yaTs = [A.bf16(256).rearrange("p (c t) -> p c t", c=2) for _ in range(2)]

    def v4(ap):
        return ap.rearrange("p (h c) -> p h c", h=4)
    for tt in range(NT):
        s = tt % 2
        ks = f"o{s}"
        rows = slice(tt * 128, (tt + 1) * 128)
        P.dma("sp", yf[s], S['yd0'][rows, :], r=["yd0"], w=[ks + "yf"])
        P.dma("sp", yb[s], S['yd1'][rows, :], r=["yd1"], w=[ks + "yb"])
        P.dma("sp", gbt[s], S['gb'][rows, :], r=["gb"], w=[ks + "gb"])
        P.dma("act", v4(vt[s]), S['rwp0'][:, rows, 448:512].rearrange("h t c -> t h c"), r=["rwp0"], w=[ks + "vt"])
        y = yf[s]
        TT(P, "pool", y, yf[s], yb[s], ALU.add, r=[ks + "yf", ks + "yb"], w=[ks + "yf"])
        RED(P, st[s][:, 0:4], v4(y), ALU.add, r=[ks + "yf"], w=[ks + "s0"])
        TT(P, "pool", sq[s], y, y, ALU.mult, r=[ks + "yf"], w=[ks + "sq"])
        RED(P, st[s][:, 4:8], v4(sq[s]), ALU.add, r=[ks + "sq"], w=[ks + "s1"])
        TS(P, "dve", st[s][:, 8:12], st[s][:, 0:4], 1.0 / 64, None, ALU.mult, None, r=[ks + "s0"], w=[ks + "mean"])
        TT(P, "dve", st[s][:, 12:16], st[s][:, 8:12], st[s][:, 8:12], ALU.mult, r=[ks + "mean"], w=[ks + "m2"])
        STT(P, st[s][:, 16:20], st[s][:, 4:8], 1.0 / 64, st[s][:, 12:16], ALU.mult, ALU.subtract,
            r=[ks + "s1", ks + "m2"], w=[ks + "var"])
        ACT(P, st[s][:, 16:20], st[s][:, 16:20], AF.Sqrt, r=[ks + "var"], w=[ks + "var"], bias=64e-5)
        RECIP(P, st[s][:, 20:24], st[s][:, 16:20], r=[ks + "var"], w=[ks + "rstd"])
        TT(P, "dve", v4(y), v4(y), b3(st[s][:, 8:12], 64, 2), ALU.subtract, r=[ks + "yf", ks + "mean"],
           w=[ks + "yf"])
        TT(P, "dve", v4(y), v4(y), b3(st[s][:, 20:24], 64, 2), ALU.mult, r=[ks + "yf", ks + "rstd"], w=[ks + "yf"])
        TT(P, "pool", y, y, ROW[:, 0:256], ALU.mult, r=[ks + "yf", "ROW"], w=[ks + "yf"])
        TT(P, "pool", y, y, ROW[:, 256:512], ALU.add, r=[ks + "yf", "ROW"], w=[ks + "yf"])
        TT(P, "dve", v4(sq[s]), v4(vt[s]), b3(gbt[s][:, 256:260], 64, 2), ALU.mult, r=[ks + "vt", ks + "gb", ks + "s1"],
           w=[ks + "sq"])
        TT(P, "pool", y, y, sq[s], ALU.add, r=[ks + "yf", ks + "sq"], w=[ks + "yf"])
        TT(P, "dve", ya[s], y, gbt[s][:, 0:256], ALU.mult, r=[ks + "yf", ks + "gb"], w=[ks + "ya"])
        ty = bank_bf(K, s)
        for c in range(2):
            TR(P, ty[:, c * 128:(c + 1) * 128], ya[s][:, c * 128:(c + 1) * 128], K.ident_b[:, :],
               r=[ks + "ya", "ident_b"], w=[f"BK{s}"])
        CP(P, "act", yaTs[s], ty[:, 0:256].rearrange("p (c t) -> p c t", c=2), r=[f"BK{s}"], w=[ks + "yaTs"])
        P.dma("sp", S['yaT'][:, :, rows].rearrange("c p t -> p c t"), yaTs[s], r=[ks + "yaTs"], w=["yaT"])
    A.release(m0)


def phase_merge(K):
    P, A, I, S, l = K.P, K.A, K.I, K.S, K.l
    m0 = A.mark()
    Wa = A.bf16(2048).rearrange("p (c o) -> p c o", c=2)
    Wc = A.bf16(2048).rearrange("p (c o) -> p c o", c=2)
    Wb = A.bf16(8192).rearrange("p (h o) -> p h o", h=8)
    Wo = A.bf16(8192).rearrange("p (k o) -> p k o", k=8)
    stage_init(K)
    load_cast(K, Wa, I['w_branch_a'][l].rearrange("(c p) o -> p c o", p=128), "Wa")
    load_cast(K, Wc, I['w_branch_c'][l].rearrange("(c p) o -> p c o", p=128), "Wc", q="act")
    load_cast(K, Wb[0:64], I['w_branch_b'][l].rearrange("(h p) o -> p h o", p=64), "Wb")
    load_cast(K, Wo, I['w_out'][l].rearrange("(k p) o -> p k o", p=128), "Wo", q="act")
    Wr = A.f32(8 * 36).rearrange("p (k c) -> p k c", k=8)
    brow = A.f32(36)
    P.dma("sp", Wr[:, :, 0:4], I['moe_w_group'][l].rearrange("(k p) c -> p k c", p=128), w=["Wr"])
    P.dma("sp", Wr[:, :, 4:36], I['moe_w_expert'][l].rearrange("(k p) c -> p k c", p=128), w=["Wr"])
    P.dma("sp", brow[:, 0:4], I['moe_b_group'][l:l + 1, :].broadcast_to([128, 4]), w=["brow"])
    P.dma("sp", brow[:, 4:36], I['moe_b_expert'][l:l + 1, :].broadcast_to([128, 32]), w=["brow"])
    G1row = A.f32(2048).rearrange("p (a c) -> p a c", a=2)
    for a in range(2):
        P.dma("sp", G1row[:, a, :], S['modrow'][0, a:a + 1].rearrange("a f p -> a (f p)").broadcast_to([128, D]),
              r=["modrow"], w=["G1row"])
    gts = [A.bf16(24 * 512).rearrange("p (j t) -> p j t", j=24)] * 2
    yaT = [A.bf16(1024).rearrange("p (c t) -> p c t", c=2) for _ in range(2)]
    ycT = [A.bf16(1024).rearrange("p (c t) -> p c t", c=2) for _ in range(2)]
    ybT = [A.bf16(4096).rearrange("p (h t) -> p h t", h=8) for _ in range(2)]
    mT = [A.bf16(4096).rearrange("p (k t) -> p k t", k=8) for _ in range(2)]
    t1 = [A.f32(512) for _ in range(2)]
    t2 = [A.f32(512) for _ in range(2)]
    t3 = [A.f32(512) for _ in range(2)]
    xt = [A.f32(D) for _ in range(2)]
    xm = [A.f32(D) for _ in range(2)]
    xn = [A.f32(D) for _ in range(2)]
    st = [A.f32(4) for _ in range(2)]
    h2f = [A.f32(D).rearrange("p (k t) -> p k t", k=8) for _ in range(2)]
    h2b = [A.bf16(D).rearrange("p (k t) -> p k t", k=8) for _ in range(2)]
    Ls = [A.f32(36) for _ in range(2)]
    rr = [A.f32(16) for _ in range(2)]
    ohg = [A.f32(4) for _ in range(2)]
    prod = [A.f32(32) for _ in range(2)]
    sel = [A.f32(8) for _ in range(2)]
    sel2 = [A.f32(8) for _ in range(2)]
    mk1 = [A.f32(8) for _ in range(2)]
    mk2 = [A.f32(8) for _ in range(2)]
    cw = [A.f32(8) for _ in range(2)]
    cmb = [A.f32(32) for _ in range(2)]
    bk = K.banks
    oi = 0
    ti = 0
    import os
    MSTOP = int(os.environ.get("MERGE_STOP", "99"))
    for bi, (c0, n) in enumerate(BLK[:int(os.environ.get("MERGE_NB", "9"))]):
        s = bi % 2
        kb = f"g{s}"
        P.dma("sp", gts[s][:, :, 0:n], S['gatesT'][:, :, c0:c0 + n].rearrange("j p t -> p j t"), r=["gatesT"],
              w=["gts"])
        P.dma("act", yaT[s][:, :, 0:n], S['yaT'][:, :, c0:c0 + n].rearrange("c p t -> p c t"), r=["yaT"],
              w=[kb + "ya"])
        P.dma("act", ycT[s][:, :, 0:n], S['ycT'][:, :, c0:c0 + n].rearrange("c p t -> p c t"), r=["ycT"],
              w=[kb + "yc"])
        P.dma("sp", ybT[s][0:64, :, 0:n], S['ybT'][:, :, c0:c0 + n].rearrange("h d t -> d h t"), r=["ybT"],
              w=[kb + "yb"])
        for oc in range(8 if MSTOP > 1 else 0):
            o = oi % 2
            oi += 1
            ocs = slice(oc * 128, (oc + 1) * 128)
            for c in range(2):
                MM(P, bk[2][:, 0:n], Wa[:, c, ocs], yaT[s][:, c, 0:n], c == 0, c == 1, r=["Wa", kb + "ya"], w=["BK2"])
            for h in range(8):
                MM(P, bk[3][:, 0:n], Wb[0:64, h, ocs], ybT[s][0:64, h, 0:n], h == 0, h == 7, r=["Wb", kb + "yb"],
                   w=["BK3"])
            for c in range(2):
                MM(P, bk[4][:, 0:n], Wc[:, c, ocs], ycT[s][:, c, 0:n], c == 0, c == 1, r=["Wc", kb + "yc"], w=["BK4"])
            MSUB = int(os.environ.get("MERGE_SUB", "9"))
            if MSUB <= 1:
                continue
            TT(P, "dve", t1[o][:, 0:n], bk[2][:, 0:n], gts[s][:, oc, 0:n], ALU.mult, r=["BK2", "gts"], w=[f"t1{o}"])
            TT(P, "dve", t2[o][:, 0:n], bk[3][:, 0:n], gts[s][:, 8 + oc, 0:n], ALU.mult, r=["BK3", "gts"],
               w=[f"t2{o}"])
            TT(P, "dve", t3[o][:, 0:n], bk[4][:, 0:n], gts[s][:, 16 + oc, 0:n], ALU.mult, r=["BK4", "gts"],
               w=[f"t3{o}"])
            if MSUB <= 2:
                continue
            TT(P, "dve", t1[o][:, 0:n], t1[o][:, 0:n], t2[o][:, 0:n], ALU.add, r=[f"t1{o}", f"t2{o}"], w=[f"t1{o}"])
            TT(P, "dve", mT[s][:, oc, 0:n], t1[o][:, 0:n], t3[o][:, 0:n], ALU.add, r=[f"t1{o}", f"t3{o}"],
               w=[kb + "mT"])
        if MSTOP <= 2:
            continue
        for tsub in range(n // 128):
            tt = c0 // 128 + tsub
            a = 1 if tt < 2 else 0
            u_ = ti % 2
            ti += 1
            ku = f"x{u_}"
            rows = slice(tt * 128, (tt + 1) * 128)
            tcols = slice(tsub * 128, (tsub + 1) * 128)
            P.dma("sp", xt[u_], K.xcur[rows, :], r=["xcur"], w=[ku + "xt"])
            for cg in range(2):
                cgs = slice(cg * 512, (cg + 1) * 512)
                for kc in range(8):
                    MM(P, bk[5 + cg][:, :], mT[s][:, kc, tcols], Wo[:, kc, cgs], kc == 0, kc == 7,
                       r=[kb + "mT", "Wo"], w=[f"BK{5 + cg}"])
                TT(P, "dve", xm[u_][:, cgs], bk[5 + cg][:, :], G1row[:, a, cgs], ALU.mult, r=[f"BK{5 + cg}", "G1row"],
                   w=[ku + "xm"])
            TT(P, "pool", xm[u_], xm[u_], xt[u_], ALU.add, r=[ku + "xm", ku + "xt"], w=[ku + "xm"])
            P.dma("sp", S['xmid'][rows, :], xm[u_], r=[ku + "xm"], w=["xmid"])
            if MSTOP <= 3:
                continue
            ACT(P, xn[u_], xm[u_], AF.Square, r=[ku + "xm"], w=[ku + "xn", ku + "s"], accum_out=st[u_][:, 0:1])
            ACT(P, st[u_][:, 1:2], st[u_][:, 0:1], AF.Sqrt, r=[ku + "s"], w=[ku + "s1"], scale=1.0 / D, bias=EPS)
            RECIP(P, st[u_][:, 2:3], st[u_][:, 1:2], r=[ku + "s1"], w=[ku + "s2"])
            ACT(P, xn[u_], xm[u_], AF.Identity, r=[ku + "xm", ku + "s2"], w=[ku + "xn"], scale=st[u_][:, 2:3])
            for hb in range(2):
                for q in range(4):
                    kc = hb * 4 + q
                    TR(P, bk[hb][:, q * 128:(q + 1) * 128], xn[u_][:, kc * 128:(kc + 1) * 128], K.ident_f[:, :],
                       r=[ku + "xn", "ident_f"], w=[f"BK{hb}"])
                ks4 = slice(hb * 4, hb * 4 + 4)
                TT(P, "dve", h2f[u_][:, ks4, :], bk[hb][:, :].rearrange("p (k t) -> p k t", k=4),
                   b3(K.G2[:, ks4, a], 128, 2), ALU.mult, r=[f"BK{hb}", "G2"], w=[ku + "h2f"])
            TT(P, "pool", h2f[u_], h2f[u_], b3(K.MOD[:, 24:32, a], 128, 2), ALU.add, r=[ku + "h2f", "MOD"],
               w=[ku + "h2f"])
            CP(P, "act", h2b[u_], h2f[u_], r=[ku + "h2f"], w=[ku + "h2b"])
            P.dma("sp", S['h2T'][:, :, rows].rearrange("k p t -> p k t"), h2b[u_], r=[ku + "h2b"], w=["h2T"])
            if MSTOP <= 4:
                continue
            for kc in range(8):
                MM(P, bk[7][:, 0:36], h2f[u_][:, kc, :], Wr[:, kc, :], kc == 0, kc == 7, r=[ku + "h2f", "Wr"], w=["BK7"])
            L, R = Ls[u_], rr[u_]
            kr_ = ku + "r"
            TT(P, "dve", L, bk[7][:, 0:36], brow, ALU.add, r=["BK7", "brow"], w=[kr_])
            RED(P, R[:, 0:1], L[:, 0:4], ALU.max, r=[kr_], w=[kr_])
            TS(P, "dve", R[:, 1:2], R[:, 0:1], -1.0, None, ALU.mult, None, r=[kr_], w=[kr_])
            ACT(P, prod[u_][:, 0:4], L[:, 0:4], AF.Exp, r=[kr_], w=[kr_], bias=R[:, 1:2], accum_out=R[:, 2:3])
            RECIP(P, R[:, 3:4], R[:, 2:3], r=[kr_], w=[kr_])
            TS(P, "dve", ohg[u_], L[:, 0:4], R[:, 0:1], None, ALU.is_equal, None, r=[kr_], w=[kr_])
            TT(P, "dve", prod[u_].rearrange("p (g e) -> p g e", g=4), L[:, 4:36].rearrange("p (g e) -> p g e", g=4),
               b3(ohg[u_], 8, 2), ALU.mult, r=[kr_], w=[kr_])
            RED(P, sel[u_], prod[u_].rearrange("p (g e) -> p e g", g=4), ALU.add, r=[kr_], w=[kr_])
            RED(P, R[:, 4:5], sel[u_], ALU.max, r=[kr_], w=[kr_])
            TS(P, "dve", mk1[u_], sel[u_], R[:, 4:5], None, ALU.is_equal, None, r=[kr_], w=[kr_])
            STT(P, sel2[u_], mk1[u_], -1e30, sel[u_], ALU.mult, ALU.add, r=[kr_], w=[kr_])
            RED(P, R[:, 5:6], sel2[u_], ALU.max, r=[kr_], w=[kr_])
            TS(P, "dve", mk2[u_], sel2[u_], R[:, 5:6], None, ALU.is_equal, None, r=[kr_], w=[kr_])
            TT(P, "dve", R[:, 6:7], R[:, 5:6], R[:, 4:5], ALU.subtract, r=[kr_], w=[kr_])
            ACT(P, R[:, 7:8], R[:, 6:7], AF.Exp, r=[kr_], w=[kr_])
            TS(P, "dve", R[:, 8:9], R[:, 7:8], 1.0, None, ALU.add, None, r=[kr_], w=[kr_])
            RECIP(P, R[:, 9:10], R[:, 8:9], r=[kr_], w=[kr_])
            TT(P, "dve", R[:, 10:11], R[:, 3:4], R[:, 9:10], ALU.mult, r=[kr_], w=[kr_])
            TT(P, "dve", R[:, 11:12], R[:, 10:11], R[:, 7:8], ALU.mult, r=[kr_], w=[kr_])
            TS(P, "dve", cw[u_], mk1[u_], R[:, 10:11], None, ALU.mult, None, r=[kr_], w=[kr_])
            STT(P, cw[u_], mk2[u_], R[:, 11:12], cw[u_], ALU.mult, ALU.add, r=[kr_], w=[kr_])
            TT(P, "dve", cmb[u_].rearrange("p (g e) -> p g e", g=4), b3(ohg[u_], 8, 2), b3(cw[u_], 4, 1), ALU.mult,
               r=[kr_], w=[ku + "cmb"])
            P.dma("sp", S['comb'][rows, :], cmb[u_], r=[ku + "cmb"], w=["comb"])
    A.release(m0)


def phase_moe(K):
    P, A, I, S, l = K.P, K.A, K.I, K.S, K.l
    last = (l == K.n_layers - 1)
    m0 = A.mark()
    HT = 17
    HTOK = HT * 128
    G2row = A.f32(2048).rearrange("p (a c) -> p a c", a=2)
    for a in range(2):
        P.dma("sp", G2row[:, a, :], S['modrow'][1, a:a + 1].rearrange("a f p -> a (f p)").broadcast_to([128, D]),
              r=["modrow"], w=["G2row"])
    h2h = A.bf16(8 * HTOK).rearrange("p (k t) -> p k t", k=8)
    yacc = A.f32(HT * D).rearrange("p (t c) -> p t c", t=HT)
    combh = A.f32(HT * 32).rearrange("p (t e) -> p t e", t=HT)
    Wg = [A.bf16(4096).rearrange("p (k c) -> p k c", k=8) for _ in range(2)]
    Wu = [A.bf16(4096).rearrange("p (k c) -> p k c", k=8) for _ in range(2)]
    Wd = [A.bf16(4096).rearrange("p (k c) -> p k c", k=4) for _ in range(1)]
    hid = [A.bf16(2048).rearrange("p (k t) -> p k t", k=4) for _ in range(2)]
    sg = [A.bf16(512) for _ in range(2)]
    xm = [A.f32(D)] * 2
    fst = [A.f32(4) for _ in range(2)]
    stage_init(K)
    if last:
        fnw = A.f32(D)
        P.dma("sp", fnw, I['final_norm_w'].rearrange("(a c) -> a c", a=1).broadcast_to([128, D]), w=["fnw"])
    bk = K.banks
    blocks = [(i * 512, 512) for i in range(4)] + [(2048, 128)]
    gi = 0
    di = 0
    hi = 0
    for half in range(2):
        tbase = half * HT
        tok0 = tbase * 128
        kh = f"H{half}"
        for kc in range(8):
            P.dma("sp", h2h[:, kc, :], S['h2T'][kc, :, tok0:tok0 + HTOK], r=["h2T"], w=["h2h"])
        P.dma("sp", combh, S['comb'][tok0:tok0 + HTOK, :].rearrange("(t p) e -> p t e", p=128), r=["comb"],
              w=["combh"])
        MEMSET(P, "pool", yacc, 0.0, r=[], w=["yacc"])
        import os
        MOE_NE = int(os.environ.get("MOE_NE", "32"))
        MOE_STOP = int(os.environ.get("MOE_STOP", "9"))
        for e in range(MOE_NE):
            s = e % 2
            load_cast(K, Wg[s], I['moe_w_gate'][l, e].rearrange("(k p) c -> p k c", p=128), f"Wg{s}")
            load_cast(K, Wu[s], I['moe_w_up'][l, e].rearrange("(k p) c -> p k c", p=128), f"Wu{s}")
            load_cast(K, Wd[0], I['moe_w_down'][l, e].rearrange("(k p) c -> p k c", p=128), "Wd0")
            for (b0, bn) in (blocks if MOE_STOP > 1 else []):
                hb = hi % 2
                hi += 1
                for hc in range(4):
                    g_ = gi % 2
                    gi += 1
                    hcs = slice(hc * 128, (hc + 1) * 128)
                    for kc in range(8):
                        MM(P, bk[g_][:, 0:bn], Wg[s][:, kc, hcs], h2h[:, kc, b0:b0 + bn], kc == 0, kc == 7,
                           r=[f"Wg{s}", "h2h"], w=[f"BK{g_}"])
                    for kc in range(8):
                        MM(P, bk[2 + g_][:, 0:bn], Wu[s][:, kc, hcs], h2h[:, kc, b0:b0 + bn], kc == 0, kc == 7,
                           r=[f"Wu{s}", "h2h"], w=[f"BK{2 + g_}"])
                    ACT(P, sg[g_][:, 0:bn], bk[g_][:, 0:bn], AF.Silu, r=[f"BK{g_}"], w=[f"sg{g_}"])
                    TT(P, "dve", hid[hb][:, hc, 0:bn], bk[2 + g_][:, 0:bn], sg[g_][:, 0:bn], ALU.mult,
                       r=[f"BK{2 + g_}", f"sg{g_}"], w=[f"hid{hb}"])
                for tsub in range(bn // 128 if MOE_STOP > 2 else 0):
                    t = (b0 // 128) + tsub
                    tcols = slice(tsub * 128, (tsub + 1) * 128)
                    for cg in range(2):
                        d_ = 4 + di % 4
                        di += 1
                        cgs = slice(cg * 512, (cg + 1) * 512)
                        for hc in range(4):
                            MM(P, bk[d_][:, :], hid[hb][:, hc, tcols], Wd[0][:, hc, cgs], hc == 0, hc == 3,
                               r=[f"hid{hb}", "Wd0"], w=[f"BK{d_}"])
                        STT(P, yacc[:, t, cgs], bk[d_][:, :], combh[:, t, e:e + 1], yacc[:, t, cgs], ALU.mult, ALU.add,
                            r=[f"BK{d_}", "combh", "yacc"], w=["yacc"])
        for t in range(HT):
            tt = tbase + t
            a = 1 if tt < 2 else 0
            u_ = t % 2
            ku = "f0"
            rows = slice(tt * 128, (tt + 1) * 128)
            if last and tt < 2:
                continue
            P.dma("sp", xm[u_], S['xmid'][rows, :], r=["xmid"], w=[ku + "xm"])
            TT(P, "dve", yacc[:, t, :], yacc[:, t, :], G2row[:, a, :], ALU.mult, r=["yacc", "G2row"], w=["yacc"])
            TT(P, "dve", xm[u_], xm[u_], yacc[:, t, :], ALU.add, r=["yacc", ku + "xm"], w=[ku + "xm"])
            if not last:
                P.dma("sp", S['xnext'][rows, :], xm[u_], r=[ku + "xm"], w=["xnext"])
            else:
                jk = yacc[:, t, :]
                ACT(P, jk, xm[u_], AF.Square, r=[ku + "xm"], w=["yacc", ku + "s"], accum_out=fst[u_][:, 0:1])
                ACT(P, fst[u_][:, 1:2], fst[u_][:, 0:1], AF.Sqrt, r=[ku + "s"], w=[ku + "s1"], scale=1.0 / D, bias=EPS)
                RECIP(P, fst[u_][:, 2:3], fst[u_][:, 1:2], r=[ku + "s1"], w=[ku + "s2"])
                STT(P, xm[u_], xm[u_], fst[u_][:, 2:3], fnw, ALU.mult, ALU.mult, r=[ku + "xm", ku + "s2", "fnw"],
                    w=[ku + "xm"])
                P.dma("sp", K.out[(tt - 2) * 128:(tt - 1) * 128, :], xm[u_], r=[ku + "xm"], w=["out"])
        P.barrier()
    A.release(m0)


_CACHE = {}


def kernel(**inputs):
    if "nc" not in _CACHE:
        _CACHE["nc"] = build(n_layers=2)[0]
    nc = _CACHE["nc"]
    consts = make_consts()
    shared = {n: np.ascontiguousarray(inputs[n], dtype=np.float32) for n in WSHAPES}
    shared.update(consts)
    in_maps = []
    for b in range(8):
        m = dict(shared)
        m["xin"] = np.ascontiguousarray(np.concatenate([inputs["ctx"][b], inputs["x"][b]], axis=0), dtype=np.float32)
        m["cc"] = np.ascontiguousarray(np.stack([inputs["c"][b], inputs["c_ctx"]], axis=0), dtype=np.float32)
        in_maps.append(m)
    G = _CACHE.get("cores_per_launch", 2)
    outs = []
    for g0 in range(0, 8, G):
        res = run_bass_kernel_spmd(nc, in_maps[g0:g0 + G], core_ids=list(range(G)))
        outs += [np.asarray(res.results[b]["out"], dtype=np.float32) for b in range(G)]
    return np.stack(outs, axis=0)
```
